# Optimizing a Trainium2 kernel written in Bass

```python
import jax, jax.numpy as jnp
from jax import lax
import numpy as np

D_MODEL = 1024
BATCH = 8
SEQ = 2048
DEPTH = 4

CHUNK = 64
Q_BLOCK = 128
EPS = 1e-6
NEG_INF = -1e30

FOX_HEADS = 8
FOX_HEAD_DIM = 64
FOX_WIDTH = FOX_HEADS * FOX_HEAD_DIM
CONV_WIDTH = D_MODEL - FOX_WIDTH
CONV_TAPS = 3
FOX_GATE_BIAS_MEAN = 2.0
AB_IN_COLS = 3 * FOX_WIDTH + FOX_HEADS + 3 * CONV_WIDTH

HGRN_HEADS = 8
HGRN_HEAD_DIM = D_MODEL // HGRN_HEADS
HGRN_WIDTH = HGRN_HEADS * HGRN_HEAD_DIM
C_IN_COLS = 4 * HGRN_WIDTH

FFN_HIDDEN = -(-8 * D_MODEL // (3 * 256)) * 256

N_AB_LAYERS = (DEPTH + 1) // 2
N_C_LAYERS = DEPTH // 2

kernel_name = "hybrid_fox_shortconv_hgrn2_trunk"


def rms_norm(x, g):
    xf = x.astype(jnp.float32)
    y = xf * lax.rsqrt(jnp.mean(xf * xf, axis=-1, keepdims=True) + EPS)
    return (y * g.astype(jnp.float32)).astype(x.dtype)


def forgetting_attention(q, k, v, f_logit):
    S = q.shape[1]
    scale = FOX_HEAD_DIM ** -0.5
    c = jnp.cumsum(jax.nn.log_sigmoid(f_logit.astype(jnp.float32)), axis=1)
    c = jnp.transpose(c, (0, 2, 1))
    outs = []
    for blk in range(S // Q_BLOCK):
        q0 = blk * Q_BLOCK
        q1 = q0 + Q_BLOCK
        s = jnp.einsum('bqhd,bkhd->bhqk', q[:, q0:q1], k[:, :q1],
                       preferred_element_type=jnp.float32) * scale
        s = s + c[:, :, q0:q1, None] - c[:, :, None, :q1]
        mask = (q0 + jnp.arange(Q_BLOCK))[:, None] >= jnp.arange(q1)[None, :]
        s = jnp.where(mask, s, NEG_INF)
        p = jax.nn.softmax(s, axis=-1)
        outs.append(jnp.einsum('bhqk,bkhd->bqhd', p.astype(v.dtype), v[:, :q1]))
    return jnp.concatenate(outs, axis=1)


def short_gated_conv(u_b, u_c, u_x, conv_w):
    S = u_x.shape[1]
    z = u_c * u_x
    zp = jnp.pad(z, ((0, 0), (CONV_TAPS - 1, 0), (0, 0)))
    y = conv_w[0] * zp[:, 0:S]
    for j in range(1, CONV_TAPS):
        y = y + conv_w[j] * zp[:, j:j + S]
    return u_b * y


def hgrn2_chunkwise(q, k, v, log_f):
    Bn, S, H, Dk = q.shape
    Dv = v.shape[-1]
    n = S // CHUNK

    def to_chunks(a):
        return jnp.transpose(a.reshape(Bn, n, CHUNK, H, a.shape[-1]), (1, 0, 3, 2, 4))

    qc, kc, vc, gc = to_chunks(q), to_chunks(k), to_chunks(v), to_chunks(log_f)
    causal = jnp.tril(jnp.ones((CHUNK, CHUNK), dtype=bool))

    def step(state, inp):
        qi, ki, vi, gi = inp
        b = jnp.cumsum(gi, axis=2)
        o_inter = jnp.einsum('bhtd,bhde->bhte', qi * jnp.exp(b), state)
        rel = jnp.where(causal[:, :, None], b[:, :, :, None, :] - b[:, :, None, :, :], -jnp.inf)
        a = jnp.einsum('bhtd,bhsd,bhtsd->bhts', qi, ki, jnp.exp(rel))
        o = o_inter + jnp.einsum('bhts,bhse->bhte', a, vi)
        b_last = b[:, :, -1]
        k_dec = ki * jnp.exp(b_last[:, :, None, :] - b)
        state = jnp.exp(b_last)[..., None] * state + jnp.einsum('bhsd,bhse->bhde', k_dec, vi)
        return state, o

    s0 = jnp.zeros((Bn, H, Dk, Dv), jnp.float32)
    _, o = lax.scan(step, s0, (qc, kc, vc, gc))
    return jnp.transpose(o, (1, 0, 3, 2, 4)).reshape(Bn, S, H, Dv)


def fox_conv_layer(h, w_in, f_bias, conv_w, w_out):
    Bn, S, _ = h.shape
    proj = h @ w_in
    cuts = [FOX_WIDTH, 2 * FOX_WIDTH, 3 * FOX_WIDTH, 3 * FOX_WIDTH + FOX_HEADS,
            3 * FOX_WIDTH + FOX_HEADS + CONV_WIDTH, 3 * FOX_WIDTH + FOX_HEADS + 2 * CONV_WIDTH]
    q, k, v, f_logit, u_b, u_c, u_x = jnp.split(proj, cuts, axis=-1)
    heads = lambda a: a.reshape(Bn, S, FOX_HEADS, FOX_HEAD_DIM)
    a_out = forgetting_attention(heads(q), heads(k), heads(v), f_logit + f_bias)
    a_out = a_out.reshape(Bn, S, FOX_WIDTH).astype(h.dtype)
    b_out = short_gated_conv(u_b, u_c, u_x, conv_w).astype(h.dtype)
    return (jnp.concatenate([a_out, b_out], axis=-1) @ w_out).astype(h.dtype)


def hgrn2_layer(h, w_in, lower_bound, head_norm, w_out):
    Bn, S, _ = h.shape
    proj = (h @ w_in).astype(jnp.float32)
    q, f_logit, i_in, g_out = jnp.split(proj, 4, axis=-1)
    f = lower_bound + (1.0 - lower_bound) * jax.nn.sigmoid(f_logit)
    log_f = jnp.log(f)
    k = 1.0 - f
    heads = lambda a: a.reshape(Bn, S, HGRN_HEADS, HGRN_HEAD_DIM)
    o = hgrn2_chunkwise(heads(q), heads(k), heads(i_in), heads(log_f))
    o = o * lax.rsqrt(jnp.mean(o * o, axis=-1, keepdims=True) + EPS)
    o = o * head_norm.astype(jnp.float32).reshape(HGRN_HEADS, HGRN_HEAD_DIM)
    o = o.reshape(Bn, S, HGRN_WIDTH) * jax.nn.silu(g_out)
    return (o.astype(h.dtype) @ w_out).astype(h.dtype)


def swiglu(h, w_in, w_out):
    gate, up = jnp.split(h @ w_in, 2, axis=-1)
    return ((jax.nn.silu(gate) * up) @ w_out).astype(h.dtype)


def setup_inputs(seed: int = 0) -> dict:
    key = jax.random.key(seed)
    ks = jax.random.split(key, 14)
    f32 = jnp.float32

    def w(k, shape, fan_in):
        return jax.random.normal(k, shape, f32) * (fan_in ** -0.5)

    def gain(k, shape):
        return 1.0 + 0.02 * jax.random.normal(k, shape, f32)

    return {
        "x": jax.random.normal(ks[0], (BATCH, SEQ, D_MODEL), f32),
        "norm_mix": gain(ks[1], (DEPTH, D_MODEL)),
        "norm_ffn": gain(ks[2], (DEPTH, D_MODEL)),
        "final_norm": gain(ks[3], (D_MODEL,)),
        "ab_w_in": w(ks[4], (N_AB_LAYERS, D_MODEL, AB_IN_COLS), D_MODEL),
        "fox_f_bias": FOX_GATE_BIAS_MEAN + 0.5 * jax.random.normal(ks[5], (N_AB_LAYERS, FOX_HEADS), f32),
        "conv_w": w(ks[6], (N_AB_LAYERS, CONV_TAPS, CONV_WIDTH), CONV_TAPS),
        "ab_w_out": w(ks[7], (N_AB_LAYERS, D_MODEL, D_MODEL), D_MODEL),
        "c_w_in": w(ks[8], (N_C_LAYERS, D_MODEL, C_IN_COLS), D_MODEL),
        "c_lower_bounds": 0.5 * jax.random.normal(ks[9], (N_C_LAYERS, HGRN_WIDTH), f32),
        "c_head_norm": gain(ks[10], (N_C_LAYERS, HGRN_WIDTH)),
        "c_w_out": w(ks[11], (N_C_LAYERS, HGRN_WIDTH, D_MODEL), HGRN_WIDTH),
        "ffn_w_in": w(ks[12], (DEPTH, D_MODEL, 2 * FFN_HIDDEN), D_MODEL),
        "ffn_w_out": w(ks[13], (DEPTH, FFN_HIDDEN, D_MODEL), FFN_HIDDEN),
    }


def reference(x, norm_mix, norm_ffn, final_norm, ab_w_in, fox_f_bias, conv_w, ab_w_out,
              c_w_in, c_lower_bounds, c_head_norm, c_w_out, ffn_w_in, ffn_w_out):
    lb = jax.nn.softmax(c_lower_bounds.astype(jnp.float32), axis=0)
    lb = jnp.cumsum(lb, axis=0) - lb[0]
    for layer in range(DEPTH):
        h = rms_norm(x, norm_mix[layer])
        j = layer // 2
        if layer % 2 == 0:
            mix = fox_conv_layer(h, ab_w_in[j], fox_f_bias[j], conv_w[j], ab_w_out[j])
        else:
            mix = hgrn2_layer(h, c_w_in[j], lb[j], c_head_norm[j], c_w_out[j])
        x = x + mix.astype(x.dtype)
        h = rms_norm(x, norm_ffn[layer])
        x = x + swiglu(h, ffn_w_in[layer], ffn_w_out[layer]).astype(x.dtype)
    return rms_norm(x, final_norm)
```

```python
import contextlib
import numpy as np
import concourse.bass as bass
import concourse.mybir as mybir
from concourse.bass_utils import run_bass_kernel_spmd

F32 = mybir.dt.float32
BF16 = mybir.dt.bfloat16
AF = mybir.ActivationFunctionType
ALU = mybir.AluOpType

S = 2048
D = 1024
NKC = 8
DEPTH = 4
FFN_H = 2816
NHC = 22
EPS = 1e-6
NCORES = 8
RING_SLOTS = 3
SLOT_ELEMS = 3072
NUNITS = 21
NEG = -30000.0

C_GMIX = 0
C_GFFN = 32
C_GFIN = 64
C_CONVW = 72
C_CLB = 96
C_HNORM = 112
C_FBIAS = 128
NSMALL = 136
DV_LB = 0
DV_OML = 16
DV_NOML = 32
DV_NFB = 48
NDERIV = 56
K_IDENT = 0
K_ONES = 128
K_MASKNEG = 256
K_HMASK = 384
K_SCAN = 896
NCONST = 896 + 2048


class SemObj:
    def __init__(self, nc, es, name):
        self.sem = es.enter_context(nc.semaphore(name))
        self.cnt = 0
        self.name = name


class Eng(SemObj):
    def __init__(self, nc, es, name, h, skip_self=False):
        super().__init__(nc, es, "s_" + name)
        self.h = h
        self.waited = {}
        self.skip_self = skip_self


class Res:
    __slots__ = ("name", "writers", "readers")

    def __init__(self, name):
        self.name = name
        self.writers = {}
        self.readers = {}


class Buf:
    def __init__(self, ap, res):
        self.ap = ap
        self.res = list(res)


class Tracker:
    def __init__(self):
        self.plan = False

    @staticmethod
    def _merge(deps, d):
        for so, v in d.items():
            if deps.get(so, 0) < v:
                deps[so] = v

    def _wait(self, eng, reads, writes):
        deps = {}
        for r in reads:
            self._merge(deps, r.writers)
        for w in writes:
            self._merge(deps, w.writers)
            self._merge(deps, w.readers)
        for so, v in deps.items():
            if so is eng and eng.skip_self:
                continue
            if eng.waited.get(so, 0) >= v:
                continue
            eng.h.wait_ge(so.sem, v)
            eng.waited[so] = v

    def op(self, eng, fn, reads=(), writes=()):
        if self.plan:
            return
        self._wait(eng, reads, writes)
        inst = fn()
        eng.cnt += 1
        inst.then_inc(eng.sem, 1)
        for r in reads:
            r.readers[eng] = eng.cnt
        for w in writes:
            w.writers[eng] = eng.cnt

    def dma(self, q, so, fn, reads=(), writes=()):
        if self.plan:
            return
        self._wait(q, reads, writes)
        inst = fn()
        so.cnt += 16
        inst.then_inc(so.sem, 16)
        for r in reads:
            r.readers[so] = so.cnt
        for w in writes:
            w.writers[so] = so.cnt


def flat_res(*items):
    out = []
    for it in items:
        if isinstance(it, Res):
            out.append(it)
        elif isinstance(it, Buf):
            out.extend(it.res)
        else:
            for x in it:
                out.extend(flat_res(x))
    return out


def build(layers, final, n_ab, n_c, n_ffn, comps=("mix", "ffn")):
    nc = bass.Bass("TRN2", target_bir_lowering=False)
    es = contextlib.ExitStack()
    T = Tracker()

    def dram(name, shape, kind="ExternalInput"):
        return nc.dram_tensor(name, list(shape), F32, kind=kind).ap()

    xT_d = dram("xT", [D, S])
    yT_d = dram("yT", [D, S], kind="ExternalOutput")
    small_d = dram("small", [128, NSMALL])
    const_d = dram("consts", [128, NCONST])
    ab_att_d = dram("ab_att", [max(n_ab, 1), D, 4, 384])
    ab_f_d = dram("ab_f", [max(n_ab, 1), D, 8])
    ab_conv_d = dram("ab_conv", [max(n_ab, 1), D, 4, 384])
    ab_out_d = dram("ab_out", [max(n_ab, 1), D, D])
    c_in_d = dram("c_in", [max(n_c, 1), D, 8, 512])
    c_out_d = dram("c_out", [max(n_c, 1), D, D])
    ffn_in_d = dram("ffn_in", [max(n_ffn, 1), D, NHC, 256])
    ffn_out_d = dram("ffn_out", [max(n_ffn, 1), FFN_H, D])
    chl_d = nc.dram_tensor("chl_scr", [8, 3, S], BF16, kind="Internal").ap()

    with es:
        def sb(name, shape, dt):
            return es.enter_context(nc.sbuf_tensor(name, list(shape), dt))

        xT = sb("xT_sb", [128, NKC, S], F32)
        hT = sb("hT_sb", [128, NKC, S], BF16)
        ring = sb("ring", [128, RING_SLOTS, SLOT_ELEMS], BF16)
        U = sb("U", [128, NUNITS, S], BF16)
        small = sb("small_sb", [128, NSMALL], F32)
        deriv = sb("deriv_sb", [128, NDERIV], F32)
        cst = sb("cst_sb", [128, NCONST], BF16)
        recb = sb("recb", [64, 512], F32)
        tick = sb("tick", [128, 4], F32)
        Sst = sb("Sst", [128, 128], F32)
        Sbb = sb("Sbb", [128, 2, 128], BF16)
        ps = es.enter_context(nc.psum_tensor("ps", [128, 8, 512], F32))
        psf = ps[:, :, :].rearrange("p b c -> p (b c)")

        PE = Eng(nc, es, "pe", nc.tensor, skip_self=True)
        ACT = Eng(nc, es, "act", nc.scalar)
        DVE = Eng(nc, es, "dve", nc.vector)
        POOL = Eng(nc, es, "pool", nc.gpsimd)
        SP = Eng(nc, es, "sp", nc.sync)
        ring_sems = [SemObj(nc, es, f"ring{i}") for i in range(RING_SLOTS)]
        sem_xc = [SemObj(nc, es, f"xload{c}") for c in range(NKC)]
        sem_misc = SemObj(nc, es, "misc")
        sem_cst = SemObj(nc, es, "cstload")
        sem_out = SemObj(nc, es, "outst")
        sem_rows = [SemObj(nc, es, f"rows{i}") for i in range(8)]
        sem_chlw = SemObj(nc, es, "chlw")

        x_res = [Res(f"x{c}") for c in range(NKC)]
        h_res = [Res(f"h{c}") for c in range(NKC)]
        u_res = [Res(f"u{i}") for i in range(NUNITS)]
        bank = [Res(f"bank{i}") for i in range(8)]
        ring_res = [Res(f"ring{i}") for i in range(RING_SLOTS)]
        small_res = Res("small")
        deriv_res = Res("deriv")
        cst_res = Res("cst")
        S_res = Res("S")
        recb_res = Res("recb")
        chl_dram_res = Res("chl_dram")
        Sb_res = [Res("Sb0"), Res("Sb1")]

        def ubuf(u0, n, dt=BF16, parts=128, shape=None):
            ap = U[0:parts, u0:u0 + n, :].rearrange("p a b -> p (a b)")
            if dt == F32:
                ap = ap.bitcast(F32)
            if shape is not None:
                ap = ap.rearrange(shape[0], **shape[1])
            return Buf(ap, u_res[u0:u0 + n])

        ident = cst[:, K_IDENT:K_IDENT + 128]
        onesb = cst[:, K_ONES:K_ONES + 128]
        maskneg = cst[:, K_MASKNEG:K_MASKNEG + 128]
        hmask = cst[:, K_HMASK:K_HMASK + 512]
        scanmask = cst[:, K_SCAN:K_SCAN + S]

        def v4(ap):
            return ap.rearrange("p (a b) -> p a b", a=4)

        class WStream:
            def __init__(self):
                self.plan = []
                self.i_issue = 0
                self.i_get = 0

            def _issue(self, i):
                key, src, a, b = self.plan[i]
                slot = i % RING_SLOTS
                dst = ring[:, slot, 0:a * b].rearrange("p (a b) -> p a b", a=a)
                T.dma(POOL, ring_sems[slot],
                      lambda: nc.gpsimd.dma_start(out=dst, in_=src),
                      writes=[ring_res[slot]])

            def get(self, key, src, a, b, hold=0):
                if T.plan:
                    self.plan.append((key, src, a, b))
                    return Buf(ring[:, 0, 0:a * b].rearrange("p (a b) -> p a b", a=a), [ring_res[0]])
                i = self.i_get
                self.i_get += 1
                assert self.plan[i][0] == key, (self.plan[i][0], key)
                while self.i_issue < len(self.plan) and self.i_issue <= i - 1 - hold + RING_SLOTS:
                    self._issue(self.i_issue)
                    self.i_issue += 1
                slot = i % RING_SLOTS
                return Buf(ring[:, slot, 0:a * b].rearrange("p (a b) -> p a b", a=a), [ring_res[slot]])

        W = WStream()

        def proj_fm(w, c0, m, banks, m_out=None):
            for kc in range(NKC):
                def fn():
                    inst = None
                    for tt in range(4):
                        inst = nc.tensor.matmul(ps[0:m, banks[tt], :], lhsT=w.ap[:, kc, c0:c0 + m],
                                                rhs=hT[:, kc, tt * 512:(tt + 1) * 512],
                                                start=(kc == 0), stop=(kc == NKC - 1))
                    return inst
                T.op(PE, fn, reads=flat_res(w, h_res[kc]), writes=[bank[b] for b in banks])

        def rmsnorm(gcol, sq, rstd):
            for c in range(NKC):
                sqb = sq[c % 2]
                T.op(ACT, lambda: nc.scalar.activation(out=sqb.ap, in_=xT[:, c, :], func=AF.Square),
                     reads=[x_res[c]], writes=sqb.res)

                def fn():
                    inst = None
                    for tt in range(4):
                        inst = nc.tensor.matmul(ps[:, tt, :], lhsT=onesb, rhs=sqb.ap[:, tt * 512:(tt + 1) * 512],
                                                start=(c == 0), stop=(c == NKC - 1))
                    return inst
                T.op(PE, fn, reads=flat_res(sqb, cst_res), writes=bank[0:4])
            T.op(ACT, lambda: nc.scalar.activation(out=v4(rstd.ap), in_=ps[:, 0:4, :], func=AF.Ln,
                                                   scale=1.0 / D, bias=eps_ap),
                 reads=flat_res(bank[0:4], deriv_res), writes=rstd.res)
            T.op(ACT, lambda: nc.scalar.activation(out=rstd.ap, in_=rstd.ap, func=AF.Exp, scale=-0.5),
                 reads=rstd.res, writes=rstd.res)
            for c in range(NKC):
                T.op(DVE, lambda: nc.vector.scalar_tensor_tensor(
                    out=hT[:, c, :], in0=xT[:, c, :], scalar=small[:, gcol + c:gcol + c + 1],
                    in1=rstd.ap, op0=ALU.mult, op1=ALU.mult),
                    reads=flat_res(x_res[c], rstd, small_res), writes=[h_res[c]])

        def out_accum(key, src_fn, nk, rhs_list):
            for dcp in range(4):
                w = W.get((key, dcp), src_fn(dcp), nk, 256)
                for dcl in range(2):
                    dc = dcp * 2 + dcl
                    b0 = 4 * dcl

                    def fn():
                        inst = None
                        for j in range(nk):
                            for tt in range(4):
                                inst = nc.tensor.matmul(ps[:, b0 + tt, :], lhsT=w.ap[:, j, dcl * 128:(dcl + 1) * 128],
                                                        rhs=rhs_list[j].ap[:, tt * 512:(tt + 1) * 512],
                                                        start=(j == 0), stop=(j == nk - 1))
                        return inst
                    T.op(PE, fn, reads=flat_res(w, rhs_list), writes=bank[b0:b0 + 4])
                    T.op(DVE, lambda: nc.vector.tensor_tensor(out=v4(xT[:, dc, :]), in0=v4(xT[:, dc, :]),
                                                              in1=ps[:, b0:b0 + 4, :], op=ALU.add),
                         reads=flat_res(bank[b0:b0 + 4], x_res[dc]), writes=[x_res[dc]])

        def ffn(gl, fi):
            sq = [ubuf(13, 1), ubuf(14, 1)]
            rstd = ubuf(15, 2, F32)
            rmsnorm(C_GFFN + gl * 8, sq, rstd)
            actT = [ubuf(j, 1) for j in range(11)]
            stmp = [ubuf(11, 1), ubuf(12, 1)]
            for half in range(2):
                for jj in range(11):
                    hc = half * 11 + jj
                    src = ffn_in_d[fi].rearrange("(kc p) h c -> p kc h c", p=128)[:, :, hc, :]
                    w = W.get(("ffn_in", gl, hc), src, NKC, 256)
                    proj_fm(w, 0, 128, [0, 1, 2, 3])
                    proj_fm(w, 128, 128, [4, 5, 6, 7])
                    st = stmp[jj % 2]
                    T.op(ACT, lambda: nc.scalar.activation(out=v4(st.ap), in_=ps[:, 0:4, :], func=AF.Silu),
                         reads=bank[0:4], writes=st.res)
                    T.op(DVE, lambda: nc.vector.tensor_tensor(out=v4(actT[jj].ap), in0=v4(st.ap),
                                                              in1=ps[:, 4:8, :], op=ALU.mult),
                         reads=flat_res(st, bank[4:8]), writes=actT[jj].res)

                def src_fn(dcp, half=half):
                    r0 = half * 1408
                    return ffn_out_d[fi][r0:r0 + 1408, dcp * 256:(dcp + 1) * 256].rearrange(
                        "(j p) c -> p j c", p=128)
                out_accum(("ffn_out", gl, half), src_fn, 11, actT)

        def fox_layer(gl, ai):
            gj = gl // 2
            sq = [ubuf(13, 1), ubuf(14, 1)]
            rstd = ubuf(0, 2, F32)
            rmsnorm(C_GMIX + gl * 8, sq, rstd)
            bout = [ubuf(13 + j, 1) for j in range(4)]
            aout = [ubuf(17 + j, 1) for j in range(4)]

            F0 = ubuf(0, 2, F32, parts=8)
            F1 = ubuf(2, 2, F32, parts=8)
            ones1 = ubuf(4, 1, BF16, parts=8)
            chl = ubuf(5, 3, BF16, parts=8, shape=("p (a b) -> p a b", dict(a=3)))
            src = ab_f_d[ai].rearrange("(kc p) c -> p kc c", p=128)
            w = W.get(("ab_f", gl), src, NKC, 8)
            proj_fm(w, 0, 8, [0, 1, 2, 3])
            nfb = deriv[0:8, DV_NFB + gj:DV_NFB + gj + 1]
            T.op(ACT, lambda: nc.scalar.activation(out=v4(F0.ap), in_=ps[0:8, 0:4, :], func=AF.Exp,
                                                   scale=-1.0, bias=nfb),
                 reads=flat_res(bank[0:4], deriv_res), writes=F0.res)
            T.op(ACT, lambda: nc.scalar.activation(out=F0.ap, in_=F0.ap, func=AF.Ln, scale=1.0, bias=one_ap[0:8, :]),
                 reads=flat_res(F0, deriv_res), writes=F0.res)
            T.op(DVE, lambda: nc.vector.memset(ones1.ap, 1.0), writes=ones1.res)
            T.op(DVE, lambda: nc.vector.tensor_tensor_scan(out=F1.ap, data0=ones1.ap, data1=F0.ap, initial=0.0,
                                                           op0=ALU.mult, op1=ALU.subtract),
                 reads=flat_res(ones1, F0), writes=F1.res)
            T.op(DVE, lambda: nc.vector.tensor_copy(out=chl.ap[:, 0, :], in_=F1.ap), reads=F1.res, writes=chl.res)
            T.op(DVE, lambda: nc.vector.tensor_tensor(out=F0.ap, in0=F1.ap, in1=chl.ap[:, 0, :], op=ALU.subtract),
                 reads=flat_res(F1, chl), writes=F0.res)
            T.op(DVE, lambda: nc.vector.tensor_copy(out=chl.ap[:, 1, :], in_=F0.ap), reads=F0.res, writes=chl.res)
            T.op(DVE, lambda: nc.vector.tensor_tensor(out=F1.ap, in0=F0.ap, in1=chl.ap[:, 1, :], op=ALU.subtract),
                 reads=flat_res(F0, chl), writes=F1.res)
            T.op(DVE, lambda: nc.vector.tensor_copy(out=chl.ap[:, 2, :], in_=F1.ap), reads=F1.res, writes=chl.res)
            T.dma(SP, sem_chlw, lambda: nc.sync.dma_start(out=chl_d[:, :, :], in_=chl.ap), reads=chl.res,
                  writes=[chl_dram_res])

            T0 = ubuf(0, 2, F32)
            Zb = Buf(U[:, 2:5, :].rearrange("p a b -> p (a b)").bitcast(F32)[:, 0:S + 2], u_res[2:5])
            Yb = ubuf(5, 2, F32)
            T.op(DVE, lambda: nc.vector.memset(Zb.ap[:, 0:2], 0.0), writes=Zb.res)
            for j in range(4):
                gA = [0, 1, 2, 3] if j % 2 == 0 else [4, 5, 6, 7]
                gB = [4, 5, 6, 7] if j % 2 == 0 else [0, 1, 2, 3]
                a0, b0 = gA[0], gB[0]
                src = ab_conv_d[ai].rearrange("(kc p) j c -> p kc j c", p=128)[:, :, j, 0:256]
                wcx = W.get(("ab_conv_cx", gl, j), src, NKC, 256)
                proj_fm(wcx, 0, 128, gA)
                proj_fm(wcx, 128, 128, gB)
                T.op(ACT, lambda: nc.scalar.copy(out=v4(T0.ap), in_=ps[:, a0:a0 + 4, :]), reads=bank[a0:a0 + 4],
                     writes=T0.res)
                T.op(DVE, lambda: nc.vector.tensor_tensor(out=v4(Zb.ap[:, 2:S + 2]), in0=v4(T0.ap),
                                                          in1=ps[:, b0:b0 + 4, :], op=ALU.mult),
                     reads=flat_res(T0, bank[b0:b0 + 4]), writes=Zb.res)
                src = ab_conv_d[ai].rearrange("(kc p) j c -> p kc j c", p=128)[:, :, j, 256:384]
                wb = W.get(("ab_conv_b", gl, j), src, NKC, 128)
                proj_fm(wb, 0, 128, gA)
                cw = lambda tap: small[:, C_CONVW + (gj * 3 + tap) * 4 + j:C_CONVW + (gj * 3 + tap) * 4 + j + 1]
                T.op(DVE, lambda: nc.vector.tensor_scalar(out=Yb.ap, in0=Zb.ap[:, 0:S], scalar1=cw(0), scalar2=None,
                                                          op0=ALU.mult),
                     reads=flat_res(Zb, small_res), writes=Yb.res)
                T.op(DVE, lambda: nc.vector.scalar_tensor_tensor(out=Yb.ap, in0=Zb.ap[:, 1:S + 1], scalar=cw(1),
                                                                 in1=Yb.ap, op0=ALU.mult, op1=ALU.add),
                     reads=flat_res(Zb, Yb, small_res), writes=Yb.res)
                T.op(DVE, lambda: nc.vector.scalar_tensor_tensor(out=Yb.ap, in0=Zb.ap[:, 2:S + 2], scalar=cw(2),
                                                                 in1=Yb.ap, op0=ALU.mult, op1=ALU.add),
                     reads=flat_res(Zb, Yb, small_res), writes=Yb.res)
                T.op(DVE, lambda: nc.vector.tensor_tensor(out=v4(bout[j].ap), in0=v4(Yb.ap), in1=ps[:, a0:a0 + 4, :],
                                                          op=ALU.mult),
                     reads=flat_res(Yb, bank[a0:a0 + 4]), writes=bout[j].res)

            qaug = [ubuf(i, 1) for i in range(4)]
            kaug = [ubuf(4 + i, 1) for i in range(4)]
            vaug = [ubuf(8 + i, 1, shape=("p (a b) -> p a b", dict(a=16))) for i in range(4)]
            pt_res = [Res(f"pt{i}") for i in range(4)]
            PT = [Buf(U[:, 12, i * 512:(i + 1) * 512], [pt_res[i]]) for i in range(4)]
            T.op(DVE, lambda: nc.vector.memset(tick[:, 0:1], 0.0), reads=[u_res[12]], writes=pt_res + [u_res[12]])
            rb = Buf(recb[:, :], [recb_res])
            for i in range(4):
                T.op(DVE, lambda: nc.vector.memset(qaug[i].ap[64:70, :], -1.0), writes=qaug[i].res)
                T.op(DVE, lambda: nc.vector.memset(kaug[i].ap[64:70, :], 1.0), writes=kaug[i].res)
                T.op(DVE, lambda: nc.vector.memset(vaug[i].ap[:, :, 64:128], 1.0), writes=vaug[i].res)
            sbanks = [0, 1, 2]

            def pair_fillers(g):
                s2 = (g % 2) * 2
                src = ab_att_d[ai].rearrange("(kc p) g c -> p kc g c", p=128)[:, :, g, :]
                watt = W.get(("ab_att", gl, g), src, NKC, 384)
                fl = []
                for which, c0, dst in ((0, 0, qaug), (1, 128, kaug)):
                    for half in range(2):
                        for kc in range(NKC):
                            def fn(kc=kc, half=half, c0=c0):
                                def mm():
                                    inst = None
                                    for t2 in range(2):
                                        tt = half * 2 + t2
                                        inst = nc.tensor.matmul(ps[:, 5 + t2, :], lhsT=watt.ap[:, kc, c0:c0 + 128],
                                                                rhs=hT[:, kc, tt * 512:(tt + 1) * 512],
                                                                start=(kc == 0), stop=(kc == NKC - 1))
                                    return inst
                                T.op(PE, mm, reads=flat_res(watt, h_res[kc]), writes=bank[5:7])
                            fl.append(fn)
                        for hh in range(2):
                            def fe(hh=hh, half=half, which=which, dst=dst):
                                o = dst[s2 + hh].ap[0:64, half * 1024:(half + 1) * 1024].rearrange("p (a b) -> p a b", a=2)
                                i_ = ps[hh * 64:hh * 64 + 64, 5:7, :]
                                if which == 0:
                                    T.op(DVE, lambda: nc.vector.tensor_scalar(out=o, in0=i_, scalar1=0.125, scalar2=None,
                                                                              op0=ALU.mult),
                                         reads=bank[5:7], writes=dst[s2 + hh].res)
                                else:
                                    T.op(DVE, lambda: nc.vector.tensor_copy(out=o, in_=i_),
                                         reads=bank[5:7], writes=dst[s2 + hh].res)
                            fl.append(fe)
                for qd in range(4):
                    for t4 in range(4):
                        def fv(qd=qd, t4=t4):
                            tt = qd * 4 + t4

                            def mm():
                                inst = None
                                for kc in range(NKC):
                                    inst = nc.tensor.matmul(ps[:, 7, t4 * 128:(t4 + 1) * 128],
                                                            lhsT=hT[:, kc, tt * 128:(tt + 1) * 128],
                                                            rhs=watt.ap[:, kc, 256:384],
                                                            start=(kc == 0), stop=(kc == NKC - 1))
                                return inst
                            T.op(PE, mm, reads=flat_res(watt, h_res), writes=[bank[7]])
                        fl.append(fv)
                    for hh in range(2):
                        def fve(qd=qd, hh=hh):
                            o = vaug[s2 + hh].ap[:, qd * 4:(qd + 1) * 4, 0:64]
                            i_ = ps[:, 7, :].rearrange("p (a b) -> p a b", a=4)[:, :, hh * 64:(hh + 1) * 64]
                            T.op(DVE, lambda: nc.vector.tensor_copy(out=o, in_=i_), reads=[bank[7]],
                                 writes=vaug[s2 + hh].res)
                        fl.append(fve)
                for hh in range(2):
                    def fr(hh=hh):
                        h = 2 * g + hh
                        T.dma(SP, sem_rows[(s2 + hh) * 2],
                              lambda: nc.sync.dma_start(out=qaug[s2 + hh].ap[64:67, :], in_=chl_d[h, :, :]),
                              reads=[chl_dram_res], writes=qaug[s2 + hh].res)
                        T.dma(SP, sem_rows[(s2 + hh) * 2 + 1],
                              lambda: nc.sync.dma_start(out=kaug[s2 + hh].ap[67:70, :], in_=chl_d[h, :, :]),
                              reads=[chl_dram_res], writes=kaug[s2 + hh].res)
                    fl.append(fr)
                return fl

            steps = []
            for qt in range(4):
                for j in range(4 * qt + 4):
                    r = j - 4 * qt
                    steps.append((qt, j, 128 * r if r > 0 else 0, r >= 0))

            for f in pair_fillers(0):
                f()
            for g in range(4):
                fillers = pair_fillers(g + 1) if g < 3 else []
                for hh in range(2):
                    h = 2 * g + hh
                    i = (g % 2) * 2 + hh
                    deferred = []

                    def emit_s(si):
                        qt, j, off, diag = steps[si]
                        sbk = sbanks[si % 3]

                        def fn():
                            inst = nc.tensor.matmul(ps[:, sbk, off:512], lhsT=kaug[i].ap[0:70, j * 128:(j + 1) * 128],
                                                    rhs=qaug[i].ap[0:70, qt * 512 + off:(qt + 1) * 512],
                                                    start=True, stop=(not diag))
                            if diag:
                                inst = nc.tensor.matmul(ps[:, sbk, off:off + 128], lhsT=ident, rhs=maskneg,
                                                        start=False, stop=True)
                            return inst
                        T.op(PE, fn, reads=flat_res(kaug[i], qaug[i], cst_res), writes=[bank[sbk]])

                    def emit_norm(qt):
                        ob = 3 + qt % 2
                        T.op(ACT, lambda: nc.scalar.activation(out=rb.ap, in_=ps[64:128, ob, :], func=AF.Ln),
                             reads=[bank[ob]], writes=rb.res)
                        T.op(ACT, lambda: nc.scalar.activation(out=rb.ap, in_=rb.ap, func=AF.Exp, scale=-1.0),
                             reads=rb.res, writes=rb.res)
                        T.op(DVE, lambda: nc.vector.tensor_tensor(
                            out=aout[h // 2].ap[(h % 2) * 64:(h % 2) * 64 + 64, qt * 512:(qt + 1) * 512],
                            in0=ps[0:64, ob, :], in1=rb.ap, op=ALU.mult),
                            reads=flat_res(bank[ob], rb), writes=aout[h // 2].res)

                    emit_s(0)
                    emit_s(1)
                    for si, (qt, j, off, diag) in enumerate(steps):
                        if si + 2 < len(steps):
                            emit_s(si + 2)
                        sbk = sbanks[si % 3]
                        pt = PT[si % 4]
                        ob = 3 + qt % 2
                        T.op(ACT, lambda: nc.scalar.activation(out=pt.ap[:, off:512], in_=ps[:, sbk, off:512],
                                                               func=AF.Exp),
                             reads=[bank[sbk]], writes=pt.res)
                        if fillers:
                            fillers.pop(0)()
                        T.op(PE, lambda: nc.tensor.matmul(ps[:, ob, off:512], lhsT=vaug[i].ap[:, j, :],
                                                          rhs=pt.ap[:, off:512], start=(j == 0), stop=(j == 4 * qt + 3)),
                             reads=flat_res(pt, vaug[i]), writes=[bank[ob]])
                        deferred = [(d - 1, q) for d, q in deferred]
                        while deferred and deferred[0][0] <= 0:
                            emit_norm(deferred.pop(0)[1])
                        if j == 4 * qt + 3:
                            deferred.append((2, qt))
                    for _, q in deferred:
                        emit_norm(q)
                for f in fillers:
                    f()

            T.op(DVE, lambda: nc.vector.memset(tick[:, 0:1], 0.0), reads=pt_res, writes=pt_res + [u_res[12]])

            def src_fn(dcp):
                return ab_out_d[ai][:, dcp * 256:(dcp + 1) * 256].rearrange("(j p) c -> p j c", p=128)
            out_accum(("ab_out", gl), src_fn, 8, aout + bout)

        def hgrn_layer(gl, ci):
            sq = [ubuf(8, 1), ubuf(9, 1)]
            rstd = ubuf(0, 2, F32)
            rmsnorm(C_GMIX + gl * 8, sq, rstd)
            Ab = ubuf(0, 2, F32)
            Bb = ubuf(2, 2, F32)
            Cb = ubuf(4, 2, F32)
            Db = ubuf(6, 2, F32)
            KDT = ubuf(4, 1)
            ATm = Buf(U[:, 5, 0:1024], [u_res[5]])
            KD = ubuf(6, 1, shape=("p (a b) -> p a b", dict(a=16)))
            QT = ubuf(8, 1)
            KT = ubuf(9, 1)
            Vb = ubuf(10, 1, shape=("p (a b) -> p a b", dict(a=16)))
            Gb = ubuf(11, 1)
            o2 = ubuf(10, 1)
            Tb = ubuf(8, 2, F32)
            OT = [ubuf(12 + h, 1) for h in range(8)]
            E3 = Bb.ap.rearrange("p (c t) -> p c t", t=64)
            psb45 = psf[:, 2048:3072].bitcast(BF16)
            Qraw = ubuf(20, 1)

            def slices(w, c0, evac):
                fl = []
                for half in range(2):
                    for kc in range(NKC):
                        def fn(kc=kc, half=half):
                            def mm():
                                inst = None
                                for t2 in range(2):
                                    tt = half * 2 + t2
                                    inst = nc.tensor.matmul(ps[:, 6 + t2, :], lhsT=w.ap[:, kc, c0:c0 + 128],
                                                            rhs=hT[:, kc, tt * 512:(tt + 1) * 512],
                                                            start=(kc == 0), stop=(kc == NKC - 1))
                                return inst
                            T.op(PE, mm, reads=flat_res(w, h_res[kc]), writes=bank[6:8])
                        fl.append(fn)
                    fl.append(lambda half=half: evac(half))
                return fl

            def h2(ap, half):
                return ap[:, half * 1024:(half + 1) * 1024].rearrange("p (a b) -> p a b", a=2)

            def pre_project(hn_):
                src = c_in_d[ci].rearrange("(kc p) h c -> p kc h c", p=128)[:, :, hn_, 0:256]
                wA = W.get(("c_in_A", gl, hn_), src, NKC, 256, hold=(1 if hn_ > 0 else 0))
                fl = slices(wA, 128, lambda half: T.op(
                    ACT, lambda: nc.scalar.activation(out=h2(Ab.ap, half), in_=ps[:, 6:8, :], func=AF.Sigmoid),
                    reads=bank[6:8], writes=Ab.res))
                fl += slices(wA, 0, lambda half: T.op(
                    DVE, lambda: nc.vector.tensor_copy(out=h2(Qraw.ap, half), in_=ps[:, 6:8, :]),
                    reads=bank[6:8], writes=Qraw.res))
                return fl

            for h in range(8):
                gj = gl // 2
                lb = deriv[:, DV_LB + gj * 8 + h:DV_LB + gj * 8 + h + 1]
                oml = deriv[:, DV_OML + gj * 8 + h:DV_OML + gj * 8 + h + 1]
                noml = deriv[:, DV_NOML + gj * 8 + h:DV_NOML + gj * 8 + h + 1]
                hn = small[:, C_HNORM + gj * 8 + h:C_HNORM + gj * 8 + h + 1]
                if h == 0:
                    for f in pre_project(0):
                        f()
                T.op(ACT, lambda: nc.scalar.activation(out=Bb.ap, in_=Ab.ap, func=AF.Ln, scale=oml, bias=lb),
                     reads=flat_res(Ab, deriv_res), writes=Bb.res)
                T.op(DVE, lambda: nc.vector.tensor_tensor_scan(out=Cb.ap, data0=scanmask, data1=Bb.ap, initial=0.0,
                                                               op0=ALU.mult, op1=ALU.add),
                     reads=flat_res(Bb, cst_res), writes=Cb.res)
                T.op(ACT, lambda: nc.scalar.activation(out=Bb.ap, in_=Cb.ap, func=AF.Exp),
                     reads=Cb.res, writes=Bb.res)
                T.op(ACT, lambda: nc.scalar.activation(out=Db.ap, in_=Cb.ap, func=AF.Exp, scale=-1.0),
                     reads=Cb.res, writes=Db.res)
                T.op(DVE, lambda: nc.vector.tensor_tensor(out=QT.ap, in0=Qraw.ap, in1=Bb.ap, op=ALU.mult),
                     reads=flat_res(Qraw, Bb), writes=QT.res)
                T.op(DVE, lambda: nc.vector.tensor_scalar(out=Ab.ap, in0=Ab.ap, scalar1=noml, scalar2=oml,
                                                          op0=ALU.mult, op1=ALU.add),
                     reads=flat_res(Ab, deriv_res), writes=Ab.res)
                T.op(DVE, lambda: nc.vector.tensor_tensor(out=KT.ap, in0=Ab.ap, in1=Db.ap, op=ALU.mult),
                     reads=flat_res(Ab, Db), writes=KT.res)
                T.op(DVE, lambda: nc.vector.tensor_tensor(
                    out=KDT.ap.rearrange("p (c t) -> p c t", t=64), in0=KT.ap.rearrange("p (c t) -> p c t", t=64),
                    in1=E3[:, :, 63:64].broadcast_to([128, 32, 64]), op=ALU.mult),
                    reads=flat_res(KT, Bb), writes=KDT.res)
                src = c_in_d[ci].rearrange("(kc p) h c -> p kc h c", p=128)[:, :, h, 256:512]
                wB = W.get(("c_in_B", gl, h), src, NKC, 256)

                def fnv():
                    inst = None
                    for tt in range(16):
                        for kc in range(NKC):
                            inst = nc.tensor.matmul(ps[:, 4 + tt // 4, (tt % 4) * 128:(tt % 4) * 128 + 128],
                                                    lhsT=hT[:, kc, tt * 128:(tt + 1) * 128],
                                                    rhs=wB.ap[:, kc, 0:128],
                                                    start=(kc == 0), stop=(kc == NKC - 1))
                    return inst
                T.op(PE, fnv, reads=flat_res(wB, h_res), writes=bank[4:8])
                T.op(ACT, lambda: nc.scalar.copy(out=Vb.ap.rearrange("p (a c) e -> p a (c e)", a=4), in_=ps[:, 4:8, :]),
                     reads=bank[4:8], writes=Vb.res)

                def fnt():
                    inst = None
                    for tt in range(16):
                        inst = nc.tensor.transpose(psb45[:, tt * 128:(tt + 1) * 128],
                                                   KDT.ap[:, tt * 128:(tt + 1) * 128], ident)
                    return inst
                T.op(PE, fnt, reads=flat_res(KDT, cst_res), writes=bank[4:6])
                T.op(DVE, lambda: nc.vector.tensor_copy(out=KD.ap.rearrange("p a b -> p (a b)"), in_=psb45),
                     reads=bank[4:6], writes=KD.res)

                def fna():
                    inst = None
                    for c in range(32):
                        r0 = (c % 2) * 64
                        inst = nc.tensor.matmul(ps[r0:r0 + 64, 6 + c // 16, ((c // 2) % 8) * 64:((c // 2) % 8) * 64 + 64],
                                                lhsT=KT.ap[:, c * 64:(c + 1) * 64], rhs=QT.ap[:, c * 64:(c + 1) * 64],
                                                start=True, stop=True)
                    return inst
                T.op(PE, fna, reads=flat_res(KT, QT), writes=bank[6:8])
                for a in range(2):
                    T.op(DVE, lambda: nc.vector.tensor_tensor(out=ATm.ap[:, a * 512:(a + 1) * 512], in0=ps[:, 6 + a, :],
                                                              in1=hmask, op=ALU.mult),
                         reads=flat_res(bank[6 + a], cst_res), writes=ATm.res)
                T.op(DVE, lambda: nc.vector.memset(Sst[:, :], 0.0), writes=[S_res])
                fillers = slices(wB, 128, lambda half: T.op(
                    ACT, lambda: nc.scalar.activation(out=h2(Gb.ap, half), in_=ps[:, 6:8, :], func=AF.Silu),
                    reads=bank[6:8], writes=Gb.res))
                if h < 7:
                    fillers += pre_project(h + 1)
                for c in range(32):
                    tt = c // 2
                    r0 = (c % 2) * 64
                    ocol = (c % 8) * 64

                    def fno():
                        inst = nc.tensor.matmul(ps[:, c // 8, ocol:ocol + 64], lhsT=Vb.ap[r0:r0 + 64, tt, :],
                                                rhs=ATm.ap[r0:r0 + 64, tt * 64:(tt + 1) * 64],
                                                start=True, stop=(c == 0))
                        if c > 0:
                            inst = nc.tensor.matmul(ps[:, c // 8, ocol:ocol + 64], lhsT=Sbb[:, (c - 1) % 2, :],
                                                    rhs=QT.ap[:, c * 64:(c + 1) * 64], start=False, stop=True)
                        return inst
                    rd = flat_res(Vb, ATm, QT) + ([Sb_res[(c - 1) % 2]] if c > 0 else [])
                    T.op(PE, fno, reads=rd, writes=[bank[c // 8]])
                    if c < 31:
                        db = 4
                        T.op(PE, lambda: nc.tensor.matmul(ps[:, db, 0:128], lhsT=KD.ap[r0:r0 + 64, tt, :],
                                                          rhs=Vb.ap[r0:r0 + 64, tt, :], start=True, stop=True),
                             reads=flat_res(KD, Vb), writes=[bank[db]])
                        T.op(DVE, lambda: nc.vector.scalar_tensor_tensor(
                            out=Sst[:, :], in0=Sst[:, :], scalar=Bb.ap[:, c * 64 + 63:c * 64 + 64], in1=ps[:, db, 0:128],
                            op0=ALU.mult, op1=ALU.add),
                            reads=flat_res(S_res, Bb, bank[db]), writes=[S_res])
                        T.op(ACT, lambda: nc.scalar.copy(out=Sbb[:, c % 2, :], in_=Sst[:, :]),
                             reads=[S_res], writes=[Sb_res[c % 2]])
                    for _ in range(2):
                        if fillers:
                            fillers.pop(0)()
                for f in fillers:
                    f()
                T.op(ACT, lambda: nc.scalar.activation(out=v4(o2.ap), in_=ps[:, 0:4, :], func=AF.Square),
                     reads=bank[0:4], writes=o2.res)

                def fns():
                    inst = None
                    for tt in range(4):
                        inst = nc.tensor.matmul(ps[:, 4 + tt, :], lhsT=onesb, rhs=o2.ap[:, tt * 512:(tt + 1) * 512],
                                                start=True, stop=True)
                    return inst
                T.op(PE, fns, reads=flat_res(o2, cst_res), writes=bank[4:8])
                T.op(ACT, lambda: nc.scalar.activation(out=v4(Db.ap), in_=ps[:, 4:8, :], func=AF.Ln,
                                                       scale=1.0 / 128, bias=eps_ap),
                     reads=flat_res(bank[4:8], deriv_res), writes=Db.res)
                T.op(ACT, lambda: nc.scalar.activation(out=Db.ap, in_=Db.ap, func=AF.Exp, scale=-0.5),
                     reads=Db.res, writes=Db.res)
                T.op(DVE, lambda: nc.vector.scalar_tensor_tensor(out=v4(Tb.ap), in0=ps[:, 0:4, :], scalar=hn,
                                                                 in1=v4(Db.ap), op0=ALU.mult, op1=ALU.mult),
                     reads=flat_res(bank[0:4], Db, small_res), writes=Tb.res)
                T.op(DVE, lambda: nc.vector.tensor_tensor(out=OT[h].ap, in0=Tb.ap, in1=Gb.ap, op=ALU.mult),
                     reads=flat_res(Tb, Gb), writes=OT[h].res)

            def src_fn(dcp):
                return c_out_d[ci][:, dcp * 256:(dcp + 1) * 256].rearrange("(j p) c -> p j c", p=128)
            out_accum(("c_out", gl), src_fn, 8, OT)

        eps_ap = deriv[:, NDERIV - 1:NDERIV]
        one_ap = deriv[:, NDERIV - 2:NDERIV - 1]

        def setup():
            T.dma(SP, sem_misc, lambda: nc.sync.dma_start(out=small[:, :], in_=small_d[:, :]), writes=[small_res])
            T.dma(POOL, sem_cst, lambda: nc.gpsimd.dma_start(out=cst[:, :], in_=const_d[:, :]), writes=[cst_res])
            for c in range(NKC):
                T.dma(SP, sem_xc[c], lambda: nc.sync.dma_start(out=xT[:, c, :], in_=xT_d[c * 128:(c + 1) * 128, :]),
                      writes=[x_res[c]])
            T.op(DVE, lambda: nc.vector.memset(deriv[:, :], 0.0), writes=[deriv_res])
            T.op(DVE, lambda: nc.vector.memset(deriv[:, NDERIV - 1:NDERIV], EPS), writes=[deriv_res])
            T.op(DVE, lambda: nc.vector.memset(deriv[:, NDERIV - 2:NDERIV - 1], 1.0), writes=[deriv_res])
            T.op(DVE, lambda: nc.vector.tensor_tensor(out=deriv[:, DV_LB + 8:DV_LB + 16], in0=small[:, C_CLB + 8:C_CLB + 16],
                                                      in1=small[:, C_CLB:C_CLB + 8], op=ALU.subtract),
                 reads=[small_res, deriv_res], writes=[deriv_res])
            T.op(ACT, lambda: nc.scalar.activation(out=deriv[:, DV_LB + 8:DV_LB + 16], in_=deriv[:, DV_LB + 8:DV_LB + 16],
                                                   func=AF.Sigmoid),
                 reads=[deriv_res], writes=[deriv_res])
            T.op(DVE, lambda: nc.vector.tensor_scalar(out=deriv[:, DV_OML:DV_OML + 16], in0=deriv[:, DV_LB:DV_LB + 16],
                                                      scalar1=-1.0, scalar2=1.0, op0=ALU.mult, op1=ALU.add),
                 reads=[deriv_res], writes=[deriv_res])
            T.op(DVE, lambda: nc.vector.tensor_scalar(out=deriv[:, DV_NOML:DV_NOML + 16], in0=deriv[:, DV_LB:DV_LB + 16],
                                                      scalar1=1.0, scalar2=-1.0, op0=ALU.mult, op1=ALU.add),
                 reads=[deriv_res], writes=[deriv_res])
            T.op(DVE, lambda: nc.vector.tensor_scalar(out=deriv[:, DV_NFB:DV_NFB + 2], in0=small[:, C_FBIAS:C_FBIAS + 2],
                                                      scalar1=-1.0, scalar2=None, op0=ALU.mult),
                 reads=[small_res, deriv_res], writes=[deriv_res])

        def finish():
            sq = [ubuf(13, 1), ubuf(14, 1)]
            rstd = ubuf(15, 2, F32)
            if final:
                for c in range(NKC):
                    sqb = sq[c % 2]
                    T.op(ACT, lambda: nc.scalar.activation(out=sqb.ap, in_=xT[:, c, :], func=AF.Square),
                         reads=[x_res[c]], writes=sqb.res)

                    def fn():
                        inst = None
                        for tt in range(4):
                            inst = nc.tensor.matmul(ps[:, tt, :], lhsT=onesb, rhs=sqb.ap[:, tt * 512:(tt + 1) * 512],
                                                    start=(c == 0), stop=(c == NKC - 1))
                        return inst
                    T.op(PE, fn, reads=flat_res(sqb, cst_res), writes=bank[0:4])
                T.op(ACT, lambda: nc.scalar.activation(out=v4(rstd.ap), in_=ps[:, 0:4, :], func=AF.Ln,
                                                       scale=1.0 / D, bias=eps_ap),
                     reads=flat_res(bank[0:4], deriv_res), writes=rstd.res)
                T.op(ACT, lambda: nc.scalar.activation(out=rstd.ap, in_=rstd.ap, func=AF.Exp, scale=-0.5),
                     reads=rstd.res, writes=rstd.res)
                for c in range(NKC):
                    T.op(DVE, lambda: nc.vector.scalar_tensor_tensor(
                        out=xT[:, c, :], in0=xT[:, c, :], scalar=small[:, C_GFIN + c:C_GFIN + c + 1],
                        in1=rstd.ap, op0=ALU.mult, op1=ALU.mult),
                        reads=flat_res(x_res[c], rstd, small_res), writes=[x_res[c]])
            for c in range(NKC):
                T.dma(SP, sem_out, lambda: nc.sync.dma_start(out=yT_d[c * 128:(c + 1) * 128, :], in_=xT[:, c, :]),
                      reads=[x_res[c]])
            if not T.plan:
                nc.sync.wait_ge(sem_out.sem, sem_out.cnt)

        def body():
            for (gl, mi, fi) in layers:
                if "mix" in comps:
                    if gl % 2 == 0:
                        fox_layer(gl, mi)
                    else:
                        hgrn_layer(gl, mi)
                if "ffn" in comps:
                    ffn(gl, fi)

        T.plan = True
        body()
        T.plan = False
        setup()
        body()
        finish()
        assert W.i_get == len(W.plan)
    return nc


def _consts():
    c = np.zeros((128, NCONST), np.float32)
    p = np.arange(128)[:, None]
    q = np.arange(128)[None, :]
    c[:, K_IDENT:K_IDENT + 128] = (p == q)
    c[:, K_ONES:K_ONES + 128] = 1.0
    c[:, K_MASKNEG:K_MASKNEG + 128] = np.where(p > q, NEG, 0.0)
    t = np.arange(512)[None, :] % 64
    c[:, K_HMASK:K_HMASK + 512] = (t >= (p % 64))
    tt = np.arange(S)[None, :]
    c[:, K_SCAN:K_SCAN + S] = np.broadcast_to((tt % 64) != 0, (128, S))
    return c


def _pack_small(inp):
    sm = np.zeros((128, NSMALL), np.float32)
    for l in range(DEPTH):
        sm[:, C_GMIX + l * 8:C_GMIX + l * 8 + 8] = inp["norm_mix"][l].reshape(8, 128).T
        sm[:, C_GFFN + l * 8:C_GFFN + l * 8 + 8] = inp["norm_ffn"][l].reshape(8, 128).T
    sm[:, C_GFIN:C_GFIN + 8] = inp["final_norm"].reshape(8, 128).T
    for j in range(2):
        for tap in range(3):
            sm[:, C_CONVW + (j * 3 + tap) * 4:C_CONVW + (j * 3 + tap) * 4 + 4] = inp["conv_w"][j, tap].reshape(4, 128).T
        sm[:, C_CLB + j * 8:C_CLB + j * 8 + 8] = inp["c_lower_bounds"][j].reshape(8, 128).T
        sm[:, C_HNORM + j * 8:C_HNORM + j * 8 + 8] = inp["c_head_norm"][j].reshape(8, 128).T
        sm[0:8, C_FBIAS + j] = inp["fox_f_bias"][j]
    return sm


def _prep_weights(inp):
    ab = inp["ab_w_in"]
    n_ab = ab.shape[0]
    q = ab[:, :, 0:512].reshape(n_ab, D, 8, 64)
    k = ab[:, :, 512:1024].reshape(n_ab, D, 8, 64)
    v = ab[:, :, 1024:1536].reshape(n_ab, D, 8, 64)
    att = np.concatenate([q.reshape(n_ab, D, 4, 128), k.reshape(n_ab, D, 4, 128), v.reshape(n_ab, D, 4, 128)],
                         axis=3)
    f = ab[:, :, 1536:1544]
    ub = ab[:, :, 1544:2056].reshape(n_ab, D, 4, 128)
    uc = ab[:, :, 2056:2568].reshape(n_ab, D, 4, 128)
    ux = ab[:, :, 2568:3080].reshape(n_ab, D, 4, 128)
    conv = np.concatenate([uc, ux, ub], axis=3)
    cw = inp["c_w_in"]
    n_c = cw.shape[0]
    cin = cw.reshape(n_c, D, 4, 8, 128).transpose(0, 1, 3, 2, 4).reshape(n_c, D, 8, 512)
    fw = inp["ffn_w_in"]
    n_f = fw.shape[0]
    fin = fw.reshape(n_f, D, 2, NHC, 128).transpose(0, 1, 3, 2, 4).reshape(n_f, D, NHC, 256)
    c = np.ascontiguousarray
    return {
        "ab_att": c(att), "ab_f": c(f), "ab_conv": c(conv), "ab_out": c(inp["ab_w_out"]),
        "c_in": c(cin), "c_out": c(inp["c_w_out"]),
        "ffn_in": c(fin), "ffn_out": c(inp["ffn_w_out"]),
    }


_NC_CACHE = {}


def run_layers(xT_list, inp, wts, layer_ids, final, comps=("mix", "ffn")):
    n_ab = sum(1 for l in layer_ids if l % 2 == 0)
    n_c = sum(1 for l in layer_ids if l % 2 == 1)
    n_f = len(layer_ids)
    layers = []
    ia = ic = 0
    for i, l in enumerate(layer_ids):
        if l % 2 == 0:
            layers.append((l, ia, i)); ia += 1
        else:
            layers.append((l, ic, i)); ic += 1
    key = (tuple(layer_ids), final, tuple(comps))
    if key not in _NC_CACHE:
        _NC_CACHE[key] = build(layers, final, n_ab, n_c, n_f, comps)
    nc = _NC_CACHE[key]
    ab_ids = [l // 2 for l in layer_ids if l % 2 == 0] or [0]
    c_ids = [l // 2 for l in layer_ids if l % 2 == 1] or [0]
    f_ids = list(layer_ids)
    shared = {
        "small": _pack_small(inp), "consts": _consts(),
        "ab_att": np.ascontiguousarray(wts["ab_att"][ab_ids]), "ab_f": np.ascontiguousarray(wts["ab_f"][ab_ids]),
        "ab_conv": np.ascontiguousarray(wts["ab_conv"][ab_ids]), "ab_out": np.ascontiguousarray(wts["ab_out"][ab_ids]),
        "c_in": np.ascontiguousarray(wts["c_in"][c_ids]), "c_out": np.ascontiguousarray(wts["c_out"][c_ids]),
        "ffn_in": np.ascontiguousarray(wts["ffn_in"][f_ids]), "ffn_out": np.ascontiguousarray(wts["ffn_out"][f_ids]),
    }
    in_maps = [dict(shared, xT=xT_list[b]) for b in range(NCORES)]
    res = run_bass_kernel_spmd(nc, in_maps, core_ids=list(range(NCORES)))
    return [np.asarray(r["yT"]) for r in res.results]


def kernel(**inputs):
    inp = {k: np.asarray(v, dtype=np.float32) for k, v in inputs.items()}
    x = inp["x"]
    wts = _prep_weights(inp)
    xT = [np.ascontiguousarray(x[b].T) for b in range(NCORES)]
    yT = run_layers(xT, inp, wts, [0, 1, 2, 3], True)
    return np.stack([y.T for y in yT], axis=0).astype(np.float32)
```

```python
import contextlib
import numpy as np
import concourse.bass as bass
import concourse.mybir as mybir
from concourse.bass_utils import run_bass_kernel_spmd

F32 = mybir.dt.float32
BF16 = mybir.dt.bfloat16
AF = mybir.ActivationFunctionType
ALU = mybir.AluOpType

S = 2048
D = 1024
NKC = 8
DEPTH = 4
FFN_H = 2816
NHC = 22
EPS = 1e-6
NCORES = 8
RING_SLOTS = 3
SLOT_ELEMS = 3072
NUNITS = 21
NEG = -30000.0

C_GMIX = 0
C_GFFN = 32
C_GFIN = 64
C_CONVW = 72
C_CLB = 96
C_HNORM = 112
C_FBIAS = 128
NSMALL = 136
DV_LB = 0
DV_OML = 16
DV_NOML = 32
DV_NFB = 48
NDERIV = 56
K_IDENT = 0
K_ONES = 128
K_MASKNEG = 256
K_HMASK = 384
K_SCAN = 896
NCONST = 896 + 2048


class SemObj:
    def __init__(self, nc, es, name):
        self.sem = es.enter_context(nc.semaphore(name))
        self.cnt = 0
        self.name = name


class Eng(SemObj):
    def __init__(self, nc, es, name, h, skip_self=False):
        super().__init__(nc, es, "s_" + name)
        self.h = h
        self.waited = {}
        self.skip_self = skip_self


class Res:
    __slots__ = ("name", "writers", "readers")

    def __init__(self, name):
        self.name = name
        self.writers = {}
        self.readers = {}


class Buf:
    def __init__(self, ap, res):
        self.ap = ap
        self.res = list(res)


class Tracker:
    def __init__(self):
        self.plan = False

    @staticmethod
    def _merge(deps, d):
        for so, v in d.items():
            if deps.get(so, 0) < v:
                deps[so] = v

    def _wait(self, eng, reads, writes):
        deps = {}
        for r in reads:
            self._merge(deps, r.writers)
        for w in writes:
            self._merge(deps, w.writers)
            self._merge(deps, w.readers)
        for so, v in deps.items():
            if so is eng and eng.skip_self:
                continue
            if eng.waited.get(so, 0) >= v:
                continue
            eng.h.wait_ge(so.sem, v)
            eng.waited[so] = v

    def op(self, eng, fn, reads=(), writes=()):
        if self.plan:
            return
        self._wait(eng, reads, writes)
        inst = fn()
        eng.cnt += 1
        inst.then_inc(eng.sem, 1)
        for r in reads:
            r.readers[eng] = eng.cnt
        for w in writes:
            w.writers[eng] = eng.cnt

    def dma(self, q, so, fn, reads=(), writes=()):
        if self.plan:
            return
        self._wait(q, reads, writes)
        inst = fn()
        so.cnt += 16
        inst.then_inc(so.sem, 16)
        for r in reads:
            r.readers[so] = so.cnt
        for w in writes:
            w.writers[so] = so.cnt


def flat_res(*items):
    out = []
    for it in items:
        if isinstance(it, Res):
            out.append(it)
        elif isinstance(it, Buf):
            out.extend(it.res)
        else:
            for x in it:
                out.extend(flat_res(x))
    return out


def build(layers, final, n_ab, n_c, n_ffn, comps=("mix", "ffn")):
    nc = bass.Bass("TRN2", target_bir_lowering=False)
    es = contextlib.ExitStack()
    T = Tracker()

    def dram(name, shape, kind="ExternalInput"):
        return nc.dram_tensor(name, list(shape), F32, kind=kind).ap()

    xT_d = dram("xT", [D, S])
    yT_d = dram("yT", [D, S], kind="ExternalOutput")
    small_d = dram("small", [128, NSMALL])
    const_d = dram("consts", [128, NCONST])
    ab_att_d = dram("ab_att", [max(n_ab, 1), D, 4, 384])
    ab_f_d = dram("ab_f", [max(n_ab, 1), D, 8])
    ab_conv_d = dram("ab_conv", [max(n_ab, 1), D, 4, 384])
    ab_out_d = dram("ab_out", [max(n_ab, 1), D, D])
    c_in_d = dram("c_in", [max(n_c, 1), D, 8, 512])
    c_out_d = dram("c_out", [max(n_c, 1), D, D])
    ffn_in_d = dram("ffn_in", [max(n_ffn, 1), D, NHC, 256])
    ffn_out_d = dram("ffn_out", [max(n_ffn, 1), FFN_H, D])
    chl_d = nc.dram_tensor("chl_scr", [8, 3, S], BF16, kind="Internal").ap()

    with es:
        def sb(name, shape, dt):
            return es.enter_context(nc.sbuf_tensor(name, list(shape), dt))

        xT = sb("xT_sb", [128, NKC, S], F32)
        hT = sb("hT_sb", [128, NKC, S], BF16)
        ring = sb("ring", [128, RING_SLOTS, SLOT_ELEMS], BF16)
        U = sb("U", [128, NUNITS, S], BF16)
        small = sb("small_sb", [128, NSMALL], F32)
        deriv = sb("deriv_sb", [128, NDERIV], F32)
        cst = sb("cst_sb", [128, NCONST], BF16)
        recb = sb("recb", [64, 512], F32)
        tick = sb("tick", [128, 4], F32)
        Sst = sb("Sst", [128, 128], F32)
        Sbb = sb("Sbb", [128, 2, 128], BF16)
        ps = es.enter_context(nc.psum_tensor("ps", [128, 8, 512], F32))
        psf = ps[:, :, :].rearrange("p b c -> p (b c)")

        PE = Eng(nc, es, "pe", nc.tensor, skip_self=True)
        ACT = Eng(nc, es, "act", nc.scalar)
        DVE = Eng(nc, es, "dve", nc.vector)
        POOL = Eng(nc, es, "pool", nc.gpsimd)
        SP = Eng(nc, es, "sp", nc.sync)
        ring_sems = [SemObj(nc, es, f"ring{i}") for i in range(RING_SLOTS)]
        sem_xc = [SemObj(nc, es, f"xload{c}") for c in range(NKC)]
        sem_misc = SemObj(nc, es, "misc")
        sem_cst = SemObj(nc, es, "cstload")
        sem_out = SemObj(nc, es, "outst")
        sem_rows = [SemObj(nc, es, f"rows{i}") for i in range(8)]
        sem_chlw = SemObj(nc, es, "chlw")

        x_res = [Res(f"x{c}") for c in range(NKC)]
        h_res = [Res(f"h{c}") for c in range(NKC)]
        u_res = [Res(f"u{i}") for i in range(NUNITS)]
        bank = [Res(f"bank{i}") for i in range(8)]
        ring_res = [Res(f"ring{i}") for i in range(RING_SLOTS)]
        small_res = Res("small")
        deriv_res = Res("deriv")
        cst_res = Res("cst")
        S_res = Res("S")
        recb_res = Res("recb")
        chl_dram_res = Res("chl_dram")
        Sb_res = [Res("Sb0"), Res("Sb1")]

        def ubuf(u0, n, dt=BF16, parts=128, shape=None):
            ap = U[0:parts, u0:u0 + n, :].rearrange("p a b -> p (a b)")
            if dt == F32:
                ap = ap.bitcast(F32)
            if shape is not None:
                ap = ap.rearrange(shape[0], **shape[1])
            return Buf(ap, u_res[u0:u0 + n])

        ident = cst[:, K_IDENT:K_IDENT + 128]
        onesb = cst[:, K_ONES:K_ONES + 128]
        maskneg = cst[:, K_MASKNEG:K_MASKNEG + 128]
        hmask = cst[:, K_HMASK:K_HMASK + 512]
        scanmask = cst[:, K_SCAN:K_SCAN + S]

        def v4(ap):
            return ap.rearrange("p (a b) -> p a b", a=4)

        class WStream:
            def __init__(self):
                self.plan = []
                self.i_issue = 0
                self.i_get = 0

            def _issue(self, i):
                key, src, a, b = self.plan[i]
                slot = i % RING_SLOTS
                dst = ring[:, slot, 0:a * b].rearrange("p (a b) -> p a b", a=a)
                T.dma(POOL, ring_sems[slot],
                      lambda: nc.gpsimd.dma_start(out=dst, in_=src),
                      writes=[ring_res[slot]])

            def get(self, key, src, a, b, hold=0):
                if T.plan:
                    self.plan.append((key, src, a, b))
                    return Buf(ring[:, 0, 0:a * b].rearrange("p (a b) -> p a b", a=a), [ring_res[0]])
                i = self.i_get
                self.i_get += 1
                assert self.plan[i][0] == key, (self.plan[i][0], key)
                while self.i_issue < len(self.plan) and self.i_issue <= i - 1 - hold + RING_SLOTS:
                    self._issue(self.i_issue)
                    self.i_issue += 1
                slot = i % RING_SLOTS
                return Buf(ring[:, slot, 0:a * b].rearrange("p (a b) -> p a b", a=a), [ring_res[slot]])

        W = WStream()

        def proj_fm(w, c0, m, banks, m_out=None):
            for kc in range(NKC):
                def fn():
                    inst = None
                    for tt in range(4):
                        inst = nc.tensor.matmul(ps[0:m, banks[tt], :], lhsT=w.ap[:, kc, c0:c0 + m],
                                                rhs=hT[:, kc, tt * 512:(tt + 1) * 512],
                                                start=(kc == 0), stop=(kc == NKC - 1))
                    return inst
                T.op(PE, fn, reads=flat_res(w, h_res[kc]), writes=[bank[b] for b in banks])

        def rmsnorm(gcol, sq, rstd):
            for c in range(NKC):
                sqb = sq[c % 2]
                T.op(ACT, lambda: nc.scalar.activation(out=sqb.ap, in_=xT[:, c, :], func=AF.Square),
                     reads=[x_res[c]], writes=sqb.res)

                def fn():
                    inst = None
                    for tt in range(4):
                        inst = nc.tensor.matmul(ps[:, tt, :], lhsT=onesb, rhs=sqb.ap[:, tt * 512:(tt + 1) * 512],
                                                start=(c == 0), stop=(c == NKC - 1))
                    return inst
                T.op(PE, fn, reads=flat_res(sqb, cst_res), writes=bank[0:4])
            T.op(ACT, lambda: nc.scalar.activation(out=v4(rstd.ap), in_=ps[:, 0:4, :], func=AF.Ln,
                                                   scale=1.0 / D, bias=eps_ap),
                 reads=flat_res(bank[0:4], deriv_res), writes=rstd.res)
            T.op(ACT, lambda: nc.scalar.activation(out=rstd.ap, in_=rstd.ap, func=AF.Exp, scale=-0.5),
                 reads=rstd.res, writes=rstd.res)
            for c in range(NKC):
                T.op(DVE, lambda: nc.vector.scalar_tensor_tensor(
                    out=hT[:, c, :], in0=xT[:, c, :], scalar=small[:, gcol + c:gcol + c + 1],
                    in1=rstd.ap, op0=ALU.mult, op1=ALU.mult),
                    reads=flat_res(x_res[c], rstd, small_res), writes=[h_res[c]])

        def out_accum(key, src_fn, nk, rhs_list):
            for dcp in range(4):
                w = W.get((key, dcp), src_fn(dcp), nk, 256)
                for dcl in range(2):
                    dc = dcp * 2 + dcl
                    b0 = 4 * dcl

                    def fn():
                        inst = None
                        for j in range(nk):
                            for tt in range(4):
                                inst = nc.tensor.matmul(ps[:, b0 + tt, :], lhsT=w.ap[:, j, dcl * 128:(dcl + 1) * 128],
                                                        rhs=rhs_list[j].ap[:, tt * 512:(tt + 1) * 512],
                                                        start=(j == 0), stop=(j == nk - 1))
                        return inst
                    T.op(PE, fn, reads=flat_res(w, rhs_list), writes=bank[b0:b0 + 4])
                    T.op(DVE, lambda: nc.vector.tensor_tensor(out=v4(xT[:, dc, :]), in0=v4(xT[:, dc, :]),
                                                              in1=ps[:, b0:b0 + 4, :], op=ALU.add),
                         reads=flat_res(bank[b0:b0 + 4], x_res[dc]), writes=[x_res[dc]])

        def ffn(gl, fi):
            sq = [ubuf(13, 1), ubuf(14, 1)]
            rstd = ubuf(15, 2, F32)
            rmsnorm(C_GFFN + gl * 8, sq, rstd)
            actT = [ubuf(j, 1) for j in range(11)]
            stmp = [ubuf(11, 1), ubuf(12, 1)]
            for half in range(2):
                for jj in range(11):
                    hc = half * 11 + jj
                    src = ffn_in_d[fi].rearrange("(kc p) h c -> p kc h c", p=128)[:, :, hc, :]
                    w = W.get(("ffn_in", gl, hc), src, NKC, 256)
                    proj_fm(w, 0, 128, [0, 1, 2, 3])
                    proj_fm(w, 128, 128, [4, 5, 6, 7])
                    st = stmp[jj % 2]
                    T.op(ACT, lambda: nc.scalar.activation(out=v4(st.ap), in_=ps[:, 0:4, :], func=AF.Silu),
                         reads=bank[0:4], writes=st.res)
                    T.op(DVE, lambda: nc.vector.tensor_tensor(out=v4(actT[jj].ap), in0=v4(st.ap),
                                                              in1=ps[:, 4:8, :], op=ALU.mult),
                         reads=flat_res(st, bank[4:8]), writes=actT[jj].res)

                def src_fn(dcp, half=half):
                    r0 = half * 1408
                    return ffn_out_d[fi][r0:r0 + 1408, dcp * 256:(dcp + 1) * 256].rearrange(
                        "(j p) c -> p j c", p=128)
                out_accum(("ffn_out", gl, half), src_fn, 11, actT)

        def fox_layer(gl, ai):
            gj = gl // 2
            sq = [ubuf(13, 1), ubuf(14, 1)]
            rstd = ubuf(0, 2, F32)
            rmsnorm(C_GMIX + gl * 8, sq, rstd)
            bout = [ubuf(13 + j, 1) for j in range(4)]
            aout = [ubuf(17 + j, 1) for j in range(4)]

            F0 = ubuf(0, 2, F32, parts=8)
            F1 = ubuf(2, 2, F32, parts=8)
            ones1 = ubuf(4, 1, BF16, parts=8)
            chl = ubuf(5, 3, BF16, parts=8, shape=("p (a b) -> p a b", dict(a=3)))
            src = ab_f_d[ai].rearrange("(kc p) c -> p kc c", p=128)
            w = W.get(("ab_f", gl), src, NKC, 8)
            proj_fm(w, 0, 8, [0, 1, 2, 3])
            nfb = deriv[0:8, DV_NFB + gj:DV_NFB + gj + 1]
            T.op(ACT, lambda: nc.scalar.activation(out=v4(F0.ap), in_=ps[0:8, 0:4, :], func=AF.Exp,
                                                   scale=-1.0, bias=nfb),
                 reads=flat_res(bank[0:4], deriv_res), writes=F0.res)
            T.op(ACT, lambda: nc.scalar.activation(out=F0.ap, in_=F0.ap, func=AF.Ln, scale=1.0, bias=one_ap[0:8, :]),
                 reads=flat_res(F0, deriv_res), writes=F0.res)
            T.op(DVE, lambda: nc.vector.memset(ones1.ap, 1.0), writes=ones1.res)
            T.op(DVE, lambda: nc.vector.tensor_tensor_scan(out=F1.ap, data0=ones1.ap, data1=F0.ap, initial=0.0,
                                                           op0=ALU.mult, op1=ALU.subtract),
                 reads=flat_res(ones1, F0), writes=F1.res)
            T.op(DVE, lambda: nc.vector.tensor_copy(out=chl.ap[:, 0, :], in_=F1.ap), reads=F1.res, writes=chl.res)
            T.op(DVE, lambda: nc.vector.tensor_tensor(out=F0.ap, in0=F1.ap, in1=chl.ap[:, 0, :], op=ALU.subtract),
                 reads=flat_res(F1, chl), writes=F0.res)
            T.op(DVE, lambda: nc.vector.tensor_copy(out=chl.ap[:, 1, :], in_=F0.ap), reads=F0.res, writes=chl.res)
            T.op(DVE, lambda: nc.vector.tensor_tensor(out=F1.ap, in0=F0.ap, in1=chl.ap[:, 1, :], op=ALU.subtract),
                 reads=flat_res(F0, chl), writes=F1.res)
            T.op(DVE, lambda: nc.vector.tensor_copy(out=chl.ap[:, 2, :], in_=F1.ap), reads=F1.res, writes=chl.res)
            T.dma(SP, sem_chlw, lambda: nc.sync.dma_start(out=chl_d[:, :, :], in_=chl.ap), reads=chl.res,
                  writes=[chl_dram_res])

            T0 = ubuf(0, 2, F32)
            Zb = Buf(U[:, 2:5, :].rearrange("p a b -> p (a b)").bitcast(F32)[:, 0:S + 2], u_res[2:5])
            Yb = ubuf(5, 2, F32)
            T.op(DVE, lambda: nc.vector.memset(Zb.ap[:, 0:2], 0.0), writes=Zb.res)
            for j in range(4):
                gA = [0, 1, 2, 3] if j % 2 == 0 else [4, 5, 6, 7]
                gB = [4, 5, 6, 7] if j % 2 == 0 else [0, 1, 2, 3]
                a0, b0 = gA[0], gB[0]
                src = ab_conv_d[ai].rearrange("(kc p) j c -> p kc j c", p=128)[:, :, j, 0:256]
                wcx = W.get(("ab_conv_cx", gl, j), src, NKC, 256)
                proj_fm(wcx, 0, 128, gA)
                proj_fm(wcx, 128, 128, gB)
                T.op(ACT, lambda: nc.scalar.copy(out=v4(T0.ap), in_=ps[:, a0:a0 + 4, :]), reads=bank[a0:a0 + 4],
                     writes=T0.res)
                T.op(DVE, lambda: nc.vector.tensor_tensor(out=v4(Zb.ap[:, 2:S + 2]), in0=v4(T0.ap),
                                                          in1=ps[:, b0:b0 + 4, :], op=ALU.mult),
                     reads=flat_res(T0, bank[b0:b0 + 4]), writes=Zb.res)
                src = ab_conv_d[ai].rearrange("(kc p) j c -> p kc j c", p=128)[:, :, j, 256:384]
                wb = W.get(("ab_conv_b", gl, j), src, NKC, 128)
                proj_fm(wb, 0, 128, gA)
                cw = lambda tap: small[:, C_CONVW + (gj * 3 + tap) * 4 + j:C_CONVW + (gj * 3 + tap) * 4 + j + 1]
                T.op(DVE, lambda: nc.vector.tensor_scalar(out=Yb.ap, in0=Zb.ap[:, 0:S], scalar1=cw(0), scalar2=None,
                                                          op0=ALU.mult),
                     reads=flat_res(Zb, small_res), writes=Yb.res)
                T.op(DVE, lambda: nc.vector.scalar_tensor_tensor(out=Yb.ap, in0=Zb.ap[:, 1:S + 1], scalar=cw(1),
                                                                 in1=Yb.ap, op0=ALU.mult, op1=ALU.add),
                     reads=flat_res(Zb, Yb, small_res), writes=Yb.res)
                T.op(DVE, lambda: nc.vector.scalar_tensor_tensor(out=Yb.ap, in0=Zb.ap[:, 2:S + 2], scalar=cw(2),
                                                                 in1=Yb.ap, op0=ALU.mult, op1=ALU.add),
                     reads=flat_res(Zb, Yb, small_res), writes=Yb.res)
                T.op(DVE, lambda: nc.vector.tensor_tensor(out=v4(bout[j].ap), in0=v4(Yb.ap), in1=ps[:, a0:a0 + 4, :],
                                                          op=ALU.mult),
                     reads=flat_res(Yb, bank[a0:a0 + 4]), writes=bout[j].res)

            qaug = [ubuf(i, 1) for i in range(4)]
            kaug = [ubuf(4 + i, 1) for i in range(4)]
            vaug = [ubuf(8 + i, 1, shape=("p (a b) -> p a b", dict(a=16))) for i in range(4)]
            pt_res = [Res(f"pt{i}") for i in range(4)]
            PT = [Buf(U[:, 12, i * 512:(i + 1) * 512], [pt_res[i]]) for i in range(4)]
            T.op(DVE, lambda: nc.vector.memset(tick[:, 0:1], 0.0), reads=[u_res[12]], writes=pt_res + [u_res[12]])
            rb = Buf(recb[:, :], [recb_res])
            for i in range(4):
                T.op(DVE, lambda: nc.vector.memset(qaug[i].ap[64:70, :], -1.0), writes=qaug[i].res)
                T.op(DVE, lambda: nc.vector.memset(kaug[i].ap[64:70, :], 1.0), writes=kaug[i].res)
                T.op(DVE, lambda: nc.vector.memset(vaug[i].ap[:, :, 64:128], 1.0), writes=vaug[i].res)
            sbanks = [0, 1, 2]

            def pair_fillers(g):
                s2 = (g % 2) * 2
                src = ab_att_d[ai].rearrange("(kc p) g c -> p kc g c", p=128)[:, :, g, :]
                watt = W.get(("ab_att", gl, g), src, NKC, 384)
                fl = []
                for which, c0, dst in ((0, 0, qaug), (1, 128, kaug)):
                    for half in range(2):
                        for kc in range(NKC):
                            def fn(kc=kc, half=half, c0=c0):
                                def mm():
                                    inst = None
                                    for t2 in range(2):
                                        tt = half * 2 + t2
                                        inst = nc.tensor.matmul(ps[:, 5 + t2, :], lhsT=watt.ap[:, kc, c0:c0 + 128],
                                                                rhs=hT[:, kc, tt * 512:(tt + 1) * 512],
                                                                start=(kc == 0), stop=(kc == NKC - 1))
                                    return inst
                                T.op(PE, mm, reads=flat_res(watt, h_res[kc]), writes=bank[5:7])
                            fl.append(fn)
                        for hh in range(2):
                            def fe(hh=hh, half=half, which=which, dst=dst):
                                o = dst[s2 + hh].ap[0:64, half * 1024:(half + 1) * 1024].rearrange("p (a b) -> p a b", a=2)
                                i_ = ps[hh * 64:hh * 64 + 64, 5:7, :]
                                if which == 0:
                                    T.op(DVE, lambda: nc.vector.tensor_scalar(out=o, in0=i_, scalar1=0.125, scalar2=None,
                                                                              op0=ALU.mult),
                                         reads=bank[5:7], writes=dst[s2 + hh].res)
                                else:
                                    T.op(DVE, lambda: nc.vector.tensor_copy(out=o, in_=i_),
                                         reads=bank[5:7], writes=dst[s2 + hh].res)
                            fl.append(fe)
                for qd in range(4):
                    for t4 in range(4):
                        def fv(qd=qd, t4=t4):
                            tt = qd * 4 + t4

                            def mm():
                                inst = None
                                for kc in range(NKC):
                                    inst = nc.tensor.matmul(ps[:, 7, t4 * 128:(t4 + 1) * 128],
                                                            lhsT=hT[:, kc, tt * 128:(tt + 1) * 128],
                                                            rhs=watt.ap[:, kc, 256:384],
                                                            start=(kc == 0), stop=(kc == NKC - 1))
                                return inst
                            T.op(PE, mm, reads=flat_res(watt, h_res), writes=[bank[7]])
                        fl.append(fv)
                    for hh in range(2):
                        def fve(qd=qd, hh=hh):
                            o = vaug[s2 + hh].ap[:, qd * 4:(qd + 1) * 4, 0:64]
                            i_ = ps[:, 7, :].rearrange("p (a b) -> p a b", a=4)[:, :, hh * 64:(hh + 1) * 64]
                            T.op(DVE, lambda: nc.vector.tensor_copy(out=o, in_=i_), reads=[bank[7]],
                                 writes=vaug[s2 + hh].res)
                        fl.append(fve)
                for hh in range(2):
                    def fr(hh=hh):
                        h = 2 * g + hh
                        T.dma(SP, sem_rows[(s2 + hh) * 2],
                              lambda: nc.sync.dma_start(out=qaug[s2 + hh].ap[64:67, :], in_=chl_d[h, :, :]),
                              reads=[chl_dram_res], writes=qaug[s2 + hh].res)
                        T.dma(SP, sem_rows[(s2 + hh) * 2 + 1],
                              lambda: nc.sync.dma_start(out=kaug[s2 + hh].ap[67:70, :], in_=chl_d[h, :, :]),
                              reads=[chl_dram_res], writes=kaug[s2 + hh].res)
                    fl.append(fr)
                return fl

            steps = []
            for qt in range(4):
                for j in range(4 * qt + 4):
                    r = j - 4 * qt
                    steps.append((qt, j, 128 * r if r > 0 else 0, r >= 0))

            for f in pair_fillers(0):
                f()
            for g in range(4):
                fillers = pair_fillers(g + 1) if g < 3 else []
                for hh in range(2):
                    h = 2 * g + hh
                    i = (g % 2) * 2 + hh
                    deferred = []

                    def emit_s(si):
                        qt, j, off, diag = steps[si]
                        sbk = sbanks[si % 3]

                        def fn():
                            inst = nc.tensor.matmul(ps[:, sbk, off:512], lhsT=kaug[i].ap[0:70, j * 128:(j + 1) * 128],
                                                    rhs=qaug[i].ap[0:70, qt * 512 + off:(qt + 1) * 512],
                                                    start=True, stop=(not diag))
                            if diag:
                                inst = nc.tensor.matmul(ps[:, sbk, off:off + 128], lhsT=ident, rhs=maskneg,
                                                        start=False, stop=True)
                            return inst
                        T.op(PE, fn, reads=flat_res(kaug[i], qaug[i], cst_res), writes=[bank[sbk]])

                    def emit_norm(qt):
                        ob = 3 + qt % 2
                        T.op(ACT, lambda: nc.scalar.activation(out=rb.ap, in_=ps[64:128, ob, :], func=AF.Ln),
                             reads=[bank[ob]], writes=rb.res)
                        T.op(ACT, lambda: nc.scalar.activation(out=rb.ap, in_=rb.ap, func=AF.Exp, scale=-1.0),
                             reads=rb.res, writes=rb.res)
                        T.op(DVE, lambda: nc.vector.tensor_tensor(
                            out=aout[h // 2].ap[(h % 2) * 64:(h % 2) * 64 + 64, qt * 512:(qt + 1) * 512],
                            in0=ps[0:64, ob, :], in1=rb.ap, op=ALU.mult),
                            reads=flat_res(bank[ob], rb), writes=aout[h // 2].res)

                    emit_s(0)
                    emit_s(1)
                    for si, (qt, j, off, diag) in enumerate(steps):
                        if si + 2 < len(steps):
                            emit_s(si + 2)
                        sbk = sbanks[si % 3]
                        pt = PT[si % 4]
                        ob = 3 + qt % 2
                        T.op(ACT, lambda: nc.scalar.activation(out=pt.ap[:, off:512], in_=ps[:, sbk, off:512],
                                                               func=AF.Exp),
                             reads=[bank[sbk]], writes=pt.res)
                        if fillers:
                            fillers.pop(0)()
                        T.op(PE, lambda: nc.tensor.matmul(ps[:, ob, off:512], lhsT=vaug[i].ap[:, j, :],
                                                          rhs=pt.ap[:, off:512], start=(j == 0), stop=(j == 4 * qt + 3)),
                             reads=flat_res(pt, vaug[i]), writes=[bank[ob]])
                        deferred = [(d - 1, q) for d, q in deferred]
                        while deferred and deferred[0][0] <= 0:
                            emit_norm(deferred.pop(0)[1])
                        if j == 4 * qt + 3:
                            deferred.append((2, qt))
                    for _, q in deferred:
                        emit_norm(q)
                for f in fillers:
                    f()

            T.op(DVE, lambda: nc.vector.memset(tick[:, 0:1], 0.0), reads=pt_res, writes=pt_res + [u_res[12]])

            def src_fn(dcp):
                return ab_out_d[ai][:, dcp * 256:(dcp + 1) * 256].rearrange("(j p) c -> p j c", p=128)
            out_accum(("ab_out", gl), src_fn, 8, aout + bout)

        def hgrn_layer(gl, ci):
            sq = [ubuf(8, 1), ubuf(9, 1)]
            rstd = ubuf(0, 2, F32)
            rmsnorm(C_GMIX + gl * 8, sq, rstd)
            Ab = ubuf(0, 2, F32)
            Bb = ubuf(2, 2, F32)
            Cb = ubuf(4, 2, F32)
            Db = ubuf(6, 2, F32)
            KDT = ubuf(4, 1)
            ATm = Buf(U[:, 5, 0:1024], [u_res[5]])
            KD = ubuf(6, 1, shape=("p (a b) -> p a b", dict(a=16)))
            QT = ubuf(8, 1)
            KT = ubuf(9, 1)
            Vb = ubuf(10, 1, shape=("p (a b) -> p a b", dict(a=16)))
            Gb = ubuf(11, 1)
            o2 = ubuf(10, 1)
            Tb = ubuf(8, 2, F32)
            OT = [ubuf(12 + h, 1) for h in range(8)]
            E3 = Bb.ap.rearrange("p (c t) -> p c t", t=64)
            psb45 = psf[:, 2048:3072].bitcast(BF16)
            Qraw = ubuf(20, 1)

            def slices(w, c0, evac):
                fl = []
                for half in range(2):
                    for kc in range(NKC):
                        def fn(kc=kc, half=half):
                            def mm():
                                inst = None
                                for t2 in range(2):
                                    tt = half * 2 + t2
                                    inst = nc.tensor.matmul(ps[:, 6 + t2, :], lhsT=w.ap[:, kc, c0:c0 + 128],
                                                            rhs=hT[:, kc, tt * 512:(tt + 1) * 512],
                                                            start=(kc == 0), stop=(kc == NKC - 1))
                                return inst
                            T.op(PE, mm, reads=flat_res(w, h_res[kc]), writes=bank[6:8])
                        fl.append(fn)
                    fl.append(lambda half=half: evac(half))
                return fl

            def h2(ap, half):
                return ap[:, half * 1024:(half + 1) * 1024].rearrange("p (a b) -> p a b", a=2)

            def pre_project(hn_):
                src = c_in_d[ci].rearrange("(kc p) h c -> p kc h c", p=128)[:, :, hn_, 0:256]
                wA = W.get(("c_in_A", gl, hn_), src, NKC, 256, hold=(1 if hn_ > 0 else 0))
                fl = slices(wA, 128, lambda half: T.op(
                    ACT, lambda: nc.scalar.activation(out=h2(Ab.ap, half), in_=ps[:, 6:8, :], func=AF.Sigmoid),
                    reads=bank[6:8], writes=Ab.res))
                fl += slices(wA, 0, lambda half: T.op(
                    DVE, lambda: nc.vector.tensor_copy(out=h2(Qraw.ap, half), in_=ps[:, 6:8, :]),
                    reads=bank[6:8], writes=Qraw.res))
                return fl

            def prep1(hh):
                gj_ = gl // 2
                lb_ = deriv[:, DV_LB + gj_ * 8 + hh:DV_LB + gj_ * 8 + hh + 1]
                oml_ = deriv[:, DV_OML + gj_ * 8 + hh:DV_OML + gj_ * 8 + hh + 1]
                T.op(ACT, lambda: nc.scalar.activation(out=Bb.ap, in_=Ab.ap, func=AF.Ln, scale=oml_, bias=lb_),
                     reads=flat_res(Ab, deriv_res), writes=Bb.res)
                T.op(DVE, lambda: nc.vector.tensor_tensor_scan(out=Cb.ap, data0=scanmask, data1=Bb.ap, initial=0.0,
                                                               op0=ALU.mult, op1=ALU.add),
                     reads=flat_res(Bb, cst_res), writes=Cb.res)

            for h in range(8):
                gj = gl // 2
                lb = deriv[:, DV_LB + gj * 8 + h:DV_LB + gj * 8 + h + 1]
                oml = deriv[:, DV_OML + gj * 8 + h:DV_OML + gj * 8 + h + 1]
                noml = deriv[:, DV_NOML + gj * 8 + h:DV_NOML + gj * 8 + h + 1]
                hn = small[:, C_HNORM + gj * 8 + h:C_HNORM + gj * 8 + h + 1]
                if h == 0:
                    for f in pre_project(0):
                        f()
                if h == 0:
                    prep1(0)
                T.op(ACT, lambda: nc.scalar.activation(out=Bb.ap, in_=Cb.ap, func=AF.Exp),
                     reads=Cb.res, writes=Bb.res)
                T.op(ACT, lambda: nc.scalar.activation(out=Db.ap, in_=Cb.ap, func=AF.Exp, scale=-1.0),
                     reads=Cb.res, writes=Db.res)
                T.op(DVE, lambda: nc.vector.tensor_tensor(out=QT.ap, in0=Qraw.ap, in1=Bb.ap, op=ALU.mult),
                     reads=flat_res(Qraw, Bb), writes=QT.res)
                T.op(DVE, lambda: nc.vector.tensor_scalar(out=Ab.ap, in0=Ab.ap, scalar1=noml, scalar2=oml,
                                                          op0=ALU.mult, op1=ALU.add),
                     reads=flat_res(Ab, deriv_res), writes=Ab.res)
                T.op(DVE, lambda: nc.vector.tensor_tensor(out=KT.ap, in0=Ab.ap, in1=Db.ap, op=ALU.mult),
                     reads=flat_res(Ab, Db), writes=KT.res)
                T.op(DVE, lambda: nc.vector.tensor_tensor(
                    out=KDT.ap.rearrange("p (c t) -> p c t", t=64), in0=KT.ap.rearrange("p (c t) -> p c t", t=64),
                    in1=E3[:, :, 63:64].broadcast_to([128, 32, 64]), op=ALU.mult),
                    reads=flat_res(KT, Bb), writes=KDT.res)
                src = c_in_d[ci].rearrange("(kc p) h c -> p kc h c", p=128)[:, :, h, 256:512]
                wB = W.get(("c_in_B", gl, h), src, NKC, 256)

                def fnv():
                    inst = None
                    for tt in range(16):
                        for kc in range(NKC):
                            inst = nc.tensor.matmul(ps[:, 4 + tt // 4, (tt % 4) * 128:(tt % 4) * 128 + 128],
                                                    lhsT=hT[:, kc, tt * 128:(tt + 1) * 128],
                                                    rhs=wB.ap[:, kc, 0:128],
                                                    start=(kc == 0), stop=(kc == NKC - 1))
                    return inst
                T.op(PE, fnv, reads=flat_res(wB, h_res), writes=bank[4:8])
                T.op(ACT, lambda: nc.scalar.copy(out=Vb.ap.rearrange("p (a c) e -> p a (c e)", a=4), in_=ps[:, 4:8, :]),
                     reads=bank[4:8], writes=Vb.res)

                def fnt():
                    inst = None
                    for tt in range(16):
                        inst = nc.tensor.transpose(psb45[:, tt * 128:(tt + 1) * 128],
                                                   KDT.ap[:, tt * 128:(tt + 1) * 128], ident)
                    return inst
                T.op(PE, fnt, reads=flat_res(KDT, cst_res), writes=bank[4:6])
                T.op(DVE, lambda: nc.vector.tensor_copy(out=KD.ap.rearrange("p a b -> p (a b)"), in_=psb45),
                     reads=bank[4:6], writes=KD.res)

                def fna():
                    inst = None
                    for c in range(32):
                        r0 = (c % 2) * 64
                        inst = nc.tensor.matmul(ps[r0:r0 + 64, 6 + c // 16, ((c // 2) % 8) * 64:((c // 2) % 8) * 64 + 64],
                                                lhsT=KT.ap[:, c * 64:(c + 1) * 64], rhs=QT.ap[:, c * 64:(c + 1) * 64],
                                                start=True, stop=True)
                    return inst
                T.op(PE, fna, reads=flat_res(KT, QT), writes=bank[6:8])
                for a in range(2):
                    T.op(DVE, lambda: nc.vector.tensor_tensor(out=ATm.ap[:, a * 512:(a + 1) * 512], in0=ps[:, 6 + a, :],
                                                              in1=hmask, op=ALU.mult),
                         reads=flat_res(bank[6 + a], cst_res), writes=ATm.res)
                T.op(DVE, lambda: nc.vector.memset(Sst[:, :], 0.0), writes=[S_res])
                fillers = slices(wB, 128, lambda half: T.op(
                    ACT, lambda: nc.scalar.activation(out=h2(Gb.ap, half), in_=ps[:, 6:8, :], func=AF.Silu),
                    reads=bank[6:8], writes=Gb.res))
                if h < 7:
                    fillers += pre_project(h + 1)
                for c in range(32):
                    tt = c // 2
                    r0 = (c % 2) * 64
                    ocol = (c % 8) * 64

                    def fno():
                        inst = nc.tensor.matmul(ps[:, c // 8, ocol:ocol + 64], lhsT=Vb.ap[r0:r0 + 64, tt, :],
                                                rhs=ATm.ap[r0:r0 + 64, tt * 64:(tt + 1) * 64],
                                                start=True, stop=(c == 0))
                        if c > 0:
                            inst = nc.tensor.matmul(ps[:, c // 8, ocol:ocol + 64], lhsT=Sbb[:, (c - 1) % 2, :],
                                                    rhs=QT.ap[:, c * 64:(c + 1) * 64], start=False, stop=True)
                        return inst
                    rd = flat_res(Vb, ATm, QT) + ([Sb_res[(c - 1) % 2]] if c > 0 else [])
                    T.op(PE, fno, reads=rd, writes=[bank[c // 8]])
                    if c < 31:
                        db = 4
                        T.op(PE, lambda: nc.tensor.matmul(ps[:, db, 0:128], lhsT=KD.ap[r0:r0 + 64, tt, :],
                                                          rhs=Vb.ap[r0:r0 + 64, tt, :], start=True, stop=True),
                             reads=flat_res(KD, Vb), writes=[bank[db]])
                        T.op(DVE, lambda: nc.vector.scalar_tensor_tensor(
                            out=Sst[:, :], in0=Sst[:, :], scalar=Bb.ap[:, c * 64 + 63:c * 64 + 64], in1=ps[:, db, 0:128],
                            op0=ALU.mult, op1=ALU.add),
                            reads=flat_res(S_res, Bb, bank[db]), writes=[S_res])
                        T.op(ACT, lambda: nc.scalar.copy(out=Sbb[:, c % 2, :], in_=Sst[:, :]),
                             reads=[S_res], writes=[Sb_res[c % 2]])
                    for _ in range(2):
                        if fillers:
                            fillers.pop(0)()
                for f in fillers:
                    f()
                if h < 7:
                    prep1(h + 1)
                T.op(ACT, lambda: nc.scalar.activation(out=v4(o2.ap), in_=ps[:, 0:4, :], func=AF.Square),
                     reads=bank[0:4], writes=o2.res)

                def fns():
                    inst = None
                    for tt in range(4):
                        inst = nc.tensor.matmul(ps[:, 4 + tt, :], lhsT=onesb, rhs=o2.ap[:, tt * 512:(tt + 1) * 512],
                                                start=True, stop=True)
                    return inst
                T.op(PE, fns, reads=flat_res(o2, cst_res), writes=bank[4:8])
                T.op(ACT, lambda: nc.scalar.activation(out=v4(Db.ap), in_=ps[:, 4:8, :], func=AF.Ln,
                                                       scale=1.0 / 128, bias=eps_ap),
                     reads=flat_res(bank[4:8], deriv_res), writes=Db.res)
                T.op(ACT, lambda: nc.scalar.activation(out=Db.ap, in_=Db.ap, func=AF.Exp, scale=-0.5),
                     reads=Db.res, writes=Db.res)
                T.op(DVE, lambda: nc.vector.scalar_tensor_tensor(out=v4(Tb.ap), in0=ps[:, 0:4, :], scalar=hn,
                                                                 in1=v4(Db.ap), op0=ALU.mult, op1=ALU.mult),
                     reads=flat_res(bank[0:4], Db, small_res), writes=Tb.res)
                T.op(DVE, lambda: nc.vector.tensor_tensor(out=OT[h].ap, in0=Tb.ap, in1=Gb.ap, op=ALU.mult),
                     reads=flat_res(Tb, Gb), writes=OT[h].res)

            def src_fn(dcp):
                return c_out_d[ci][:, dcp * 256:(dcp + 1) * 256].rearrange("(j p) c -> p j c", p=128)
            out_accum(("c_out", gl), src_fn, 8, OT)

        eps_ap = deriv[:, NDERIV - 1:NDERIV]
        one_ap = deriv[:, NDERIV - 2:NDERIV - 1]

        def setup():
            T.dma(SP, sem_misc, lambda: nc.sync.dma_start(out=small[:, :], in_=small_d[:, :]), writes=[small_res])
            T.dma(POOL, sem_cst, lambda: nc.gpsimd.dma_start(out=cst[:, :], in_=const_d[:, :]), writes=[cst_res])
            for c in range(NKC):
                T.dma(SP, sem_xc[c], lambda: nc.sync.dma_start(out=xT[:, c, :], in_=xT_d[c * 128:(c + 1) * 128, :]),
                      writes=[x_res[c]])
            T.op(DVE, lambda: nc.vector.memset(deriv[:, :], 0.0), writes=[deriv_res])
            T.op(DVE, lambda: nc.vector.memset(deriv[:, NDERIV - 1:NDERIV], EPS), writes=[deriv_res])
            T.op(DVE, lambda: nc.vector.memset(deriv[:, NDERIV - 2:NDERIV - 1], 1.0), writes=[deriv_res])
            T.op(DVE, lambda: nc.vector.tensor_tensor(out=deriv[:, DV_LB + 8:DV_LB + 16], in0=small[:, C_CLB + 8:C_CLB + 16],
                                                      in1=small[:, C_CLB:C_CLB + 8], op=ALU.subtract),
                 reads=[small_res, deriv_res], writes=[deriv_res])
            T.op(ACT, lambda: nc.scalar.activation(out=deriv[:, DV_LB + 8:DV_LB + 16], in_=deriv[:, DV_LB + 8:DV_LB + 16],
                                                   func=AF.Sigmoid),
                 reads=[deriv_res], writes=[deriv_res])
            T.op(DVE, lambda: nc.vector.tensor_scalar(out=deriv[:, DV_OML:DV_OML + 16], in0=deriv[:, DV_LB:DV_LB + 16],
                                                      scalar1=-1.0, scalar2=1.0, op0=ALU.mult, op1=ALU.add),
                 reads=[deriv_res], writes=[deriv_res])
            T.op(DVE, lambda: nc.vector.tensor_scalar(out=deriv[:, DV_NOML:DV_NOML + 16], in0=deriv[:, DV_LB:DV_LB + 16],
                                                      scalar1=1.0, scalar2=-1.0, op0=ALU.mult, op1=ALU.add),
                 reads=[deriv_res], writes=[deriv_res])
            T.op(DVE, lambda: nc.vector.tensor_scalar(out=deriv[:, DV_NFB:DV_NFB + 2], in0=small[:, C_FBIAS:C_FBIAS + 2],
                                                      scalar1=-1.0, scalar2=None, op0=ALU.mult),
                 reads=[small_res, deriv_res], writes=[deriv_res])

        def finish():
            sq = [ubuf(13, 1), ubuf(14, 1)]
            rstd = ubuf(15, 2, F32)
            if final:
                for c in range(NKC):
                    sqb = sq[c % 2]
                    T.op(ACT, lambda: nc.scalar.activation(out=sqb.ap, in_=xT[:, c, :], func=AF.Square),
                         reads=[x_res[c]], writes=sqb.res)

                    def fn():
                        inst = None
                        for tt in range(4):
                            inst = nc.tensor.matmul(ps[:, tt, :], lhsT=onesb, rhs=sqb.ap[:, tt * 512:(tt + 1) * 512],
                                                    start=(c == 0), stop=(c == NKC - 1))
                        return inst
                    T.op(PE, fn, reads=flat_res(sqb, cst_res), writes=bank[0:4])
                T.op(ACT, lambda: nc.scalar.activation(out=v4(rstd.ap), in_=ps[:, 0:4, :], func=AF.Ln,
                                                       scale=1.0 / D, bias=eps_ap),
                     reads=flat_res(bank[0:4], deriv_res), writes=rstd.res)
                T.op(ACT, lambda: nc.scalar.activation(out=rstd.ap, in_=rstd.ap, func=AF.Exp, scale=-0.5),
                     reads=rstd.res, writes=rstd.res)
                for c in range(NKC):
                    T.op(DVE, lambda: nc.vector.scalar_tensor_tensor(
                        out=xT[:, c, :], in0=xT[:, c, :], scalar=small[:, C_GFIN + c:C_GFIN + c + 1],
                        in1=rstd.ap, op0=ALU.mult, op1=ALU.mult),
                        reads=flat_res(x_res[c], rstd, small_res), writes=[x_res[c]])
            for c in range(NKC):
                T.dma(SP, sem_out, lambda: nc.sync.dma_start(out=yT_d[c * 128:(c + 1) * 128, :], in_=xT[:, c, :]),
                      reads=[x_res[c]])
            if not T.plan:
                nc.sync.wait_ge(sem_out.sem, sem_out.cnt)

        def body():
            for (gl, mi, fi) in layers:
                if "mix" in comps:
                    if gl % 2 == 0:
                        fox_layer(gl, mi)
                    else:
                        hgrn_layer(gl, mi)
                if "ffn" in comps:
                    ffn(gl, fi)

        T.plan = True
        body()
        T.plan = False
        setup()
        body()
        finish()
        assert W.i_get == len(W.plan)
    return nc


def _consts():
    c = np.zeros((128, NCONST), np.float32)
    p = np.arange(128)[:, None]
    q = np.arange(128)[None, :]
    c[:, K_IDENT:K_IDENT + 128] = (p == q)
    c[:, K_ONES:K_ONES + 128] = 1.0
    c[:, K_MASKNEG:K_MASKNEG + 128] = np.where(p > q, NEG, 0.0)
    t = np.arange(512)[None, :] % 64
    c[:, K_HMASK:K_HMASK + 512] = (t >= (p % 64))
    tt = np.arange(S)[None, :]
    c[:, K_SCAN:K_SCAN + S] = np.broadcast_to((tt % 64) != 0, (128, S))
    return c


def _pack_small(inp):
    sm = np.zeros((128, NSMALL), np.float32)
    for l in range(DEPTH):
        sm[:, C_GMIX + l * 8:C_GMIX + l * 8 + 8] = inp["norm_mix"][l].reshape(8, 128).T
        sm[:, C_GFFN + l * 8:C_GFFN + l * 8 + 8] = inp["norm_ffn"][l].reshape(8, 128).T
    sm[:, C_GFIN:C_GFIN + 8] = inp["final_norm"].reshape(8, 128).T
    for j in range(2):
        for tap in range(3):
            sm[:, C_CONVW + (j * 3 + tap) * 4:C_CONVW + (j * 3 + tap) * 4 + 4] = inp["conv_w"][j, tap].reshape(4, 128).T
        sm[:, C_CLB + j * 8:C_CLB + j * 8 + 8] = inp["c_lower_bounds"][j].reshape(8, 128).T
        sm[:, C_HNORM + j * 8:C_HNORM + j * 8 + 8] = inp["c_head_norm"][j].reshape(8, 128).T
        sm[0:8, C_FBIAS + j] = inp["fox_f_bias"][j]
    return sm


def _prep_weights(inp):
    ab = inp["ab_w_in"]
    n_ab = ab.shape[0]
    q = ab[:, :, 0:512].reshape(n_ab, D, 8, 64)
    k = ab[:, :, 512:1024].reshape(n_ab, D, 8, 64)
    v = ab[:, :, 1024:1536].reshape(n_ab, D, 8, 64)
    att = np.concatenate([q.reshape(n_ab, D, 4, 128), k.reshape(n_ab, D, 4, 128), v.reshape(n_ab, D, 4, 128)],
                         axis=3)
    f = ab[:, :, 1536:1544]
    ub = ab[:, :, 1544:2056].reshape(n_ab, D, 4, 128)
    uc = ab[:, :, 2056:2568].reshape(n_ab, D, 4, 128)
    ux = ab[:, :, 2568:3080].reshape(n_ab, D, 4, 128)
    conv = np.concatenate([uc, ux, ub], axis=3)
    cw = inp["c_w_in"]
    n_c = cw.shape[0]
    cin = cw.reshape(n_c, D, 4, 8, 128).transpose(0, 1, 3, 2, 4).reshape(n_c, D, 8, 512)
    fw = inp["ffn_w_in"]
    n_f = fw.shape[0]
    fin = fw.reshape(n_f, D, 2, NHC, 128).transpose(0, 1, 3, 2, 4).reshape(n_f, D, NHC, 256)
    c = np.ascontiguousarray
    return {
        "ab_att": c(att), "ab_f": c(f), "ab_conv": c(conv), "ab_out": c(inp["ab_w_out"]),
        "c_in": c(cin), "c_out": c(inp["c_w_out"]),
        "ffn_in": c(fin), "ffn_out": c(inp["ffn_w_out"]),
    }


_NC_CACHE = {}


def run_layers(xT_list, inp, wts, layer_ids, final, comps=("mix", "ffn")):
    n_ab = sum(1 for l in layer_ids if l % 2 == 0)
    n_c = sum(1 for l in layer_ids if l % 2 == 1)
    n_f = len(layer_ids)
    layers = []
    ia = ic = 0
    for i, l in enumerate(layer_ids):
        if l % 2 == 0:
            layers.append((l, ia, i)); ia += 1
        else:
            layers.append((l, ic, i)); ic += 1
    key = (tuple(layer_ids), final, tuple(comps))
    if key not in _NC_CACHE:
        _NC_CACHE[key] = build(layers, final, n_ab, n_c, n_f, comps)
    nc = _NC_CACHE[key]
    ab_ids = [l // 2 for l in layer_ids if l % 2 == 0] or [0]
    c_ids = [l // 2 for l in layer_ids if l % 2 == 1] or [0]
    f_ids = list(layer_ids)
    shared = {
        "small": _pack_small(inp), "consts": _consts(),
        "ab_att": np.ascontiguousarray(wts["ab_att"][ab_ids]), "ab_f": np.ascontiguousarray(wts["ab_f"][ab_ids]),
        "ab_conv": np.ascontiguousarray(wts["ab_conv"][ab_ids]), "ab_out": np.ascontiguousarray(wts["ab_out"][ab_ids]),
        "c_in": np.ascontiguousarray(wts["c_in"][c_ids]), "c_out": np.ascontiguousarray(wts["c_out"][c_ids]),
        "ffn_in": np.ascontiguousarray(wts["ffn_in"][f_ids]), "ffn_out": np.ascontiguousarray(wts["ffn_out"][f_ids]),
    }
    in_maps = [dict(shared, xT=xT_list[b]) for b in range(NCORES)]
    res = run_bass_kernel_spmd(nc, in_maps, core_ids=list(range(NCORES)))
    return [np.asarray(r["yT"]) for r in res.results]


def kernel(**inputs):
    inp = {k: np.asarray(v, dtype=np.float32) for k, v in inputs.items()}
    x = inp["x"]
    wts = _prep_weights(inp)
    xT = [np.ascontiguousarray(x[b].T) for b in range(NCORES)]
    yT = run_layers(xT, inp, wts, [0, 1, 2, 3], True)
    return np.stack([y.T for y in yT], axis=0).astype(np.float32)
```

```python
import contextlib
import numpy as np
import concourse.bass as bass
import concourse.mybir as mybir
from concourse.bass_utils import run_bass_kernel_spmd

F32 = mybir.dt.float32
BF16 = mybir.dt.bfloat16
AF = mybir.ActivationFunctionType
ALU = mybir.AluOpType

S = 2048
D = 1024
NKC = 8
DEPTH = 4
FFN_H = 2816
NHC = 22
EPS = 1e-6
NCORES = 8
RING_SLOTS = 3
SLOT_ELEMS = 3072
NUNITS = 21
NEG = -30000.0

C_GMIX = 0
C_GFFN = 32
C_GFIN = 64
C_CONVW = 72
C_CLB = 96
C_HNORM = 112
C_FBIAS = 128
NSMALL = 136
DV_LB = 0
DV_OML = 16
DV_NOML = 32
DV_NFB = 48
NDERIV = 56
K_IDENT = 0
K_ONES = 128
K_MASKNEG = 256
K_HMASK = 384
K_SCAN = 896
NCONST = 896 + 2048


class SemObj:
    def __init__(self, nc, es, name):
        self.sem = es.enter_context(nc.semaphore(name))
        self.cnt = 0
        self.name = name


class Eng(SemObj):
    def __init__(self, nc, es, name, h, skip_self=False):
        super().__init__(nc, es, "s_" + name)
        self.h = h
        self.waited = {}
        self.skip_self = skip_self


class Res:
    __slots__ = ("name", "writers", "readers")

    def __init__(self, name):
        self.name = name
        self.writers = {}
        self.readers = {}


class Buf:
    def __init__(self, ap, res):
        self.ap = ap
        self.res = list(res)


class Tracker:
    def __init__(self):
        self.plan = False

    @staticmethod
    def _merge(deps, d):
        for so, v in d.items():
            if deps.get(so, 0) < v:
                deps[so] = v

    def _wait(self, eng, reads, writes):
        deps = {}
        for r in reads:
            self._merge(deps, r.writers)
        for w in writes:
            self._merge(deps, w.writers)
            self._merge(deps, w.readers)
        for so, v in deps.items():
            if so is eng and eng.skip_self:
                continue
            if eng.waited.get(so, 0) >= v:
                continue
            eng.h.wait_ge(so.sem, v)
            eng.waited[so] = v

    def op(self, eng, fn, reads=(), writes=()):
        if self.plan:
            return
        self._wait(eng, reads, writes)
        inst = fn()
        eng.cnt += 1
        inst.then_inc(eng.sem, 1)
        for r in reads:
            r.readers[eng] = eng.cnt
        for w in writes:
            w.writers[eng] = eng.cnt

    def dma(self, q, so, fn, reads=(), writes=()):
        if self.plan:
            return
        self._wait(q, reads, writes)
        inst = fn()
        so.cnt += 16
        inst.then_inc(so.sem, 16)
        for r in reads:
            r.readers[so] = so.cnt
        for w in writes:
            w.writers[so] = so.cnt


def flat_res(*items):
    out = []
    for it in items:
        if isinstance(it, Res):
            out.append(it)
        elif isinstance(it, Buf):
            out.extend(it.res)
        else:
            for x in it:
                out.extend(flat_res(x))
    return out


def build(layers, final, n_ab, n_c, n_ffn, comps=("mix", "ffn")):
    nc = bass.Bass("TRN2", target_bir_lowering=False)
    es = contextlib.ExitStack()
    T = Tracker()

    def dram(name, shape, kind="ExternalInput"):
        return nc.dram_tensor(name, list(shape), F32, kind=kind).ap()

    xT_d = dram("xT", [D, S])
    yT_d = dram("yT", [D, S], kind="ExternalOutput")
    small_d = dram("small", [128, NSMALL])
    const_d = dram("consts", [128, NCONST])
    ab_att_d = dram("ab_att", [max(n_ab, 1), D, 4, 384])
    ab_f_d = dram("ab_f", [max(n_ab, 1), D, 8])
    ab_conv_d = dram("ab_conv", [max(n_ab, 1), D, 4, 384])
    ab_out_d = dram("ab_out", [max(n_ab, 1), D, D])
    c_in_d = dram("c_in", [max(n_c, 1), D, 8, 512])
    c_out_d = dram("c_out", [max(n_c, 1), D, D])
    ffn_in_d = dram("ffn_in", [max(n_ffn, 1), D, NHC, 256])
    ffn_out_d = dram("ffn_out", [max(n_ffn, 1), FFN_H, D])
    chl_d = nc.dram_tensor("chl_scr", [8, 3, S], BF16, kind="Internal").ap()

    with es:
        def sb(name, shape, dt):
            return es.enter_context(nc.sbuf_tensor(name, list(shape), dt))

        xT = sb("xT_sb", [128, NKC, S], F32)
        hT = sb("hT_sb", [128, NKC, S], BF16)
        ring = sb("ring", [128, RING_SLOTS, SLOT_ELEMS], BF16)
        U = sb("U", [128, NUNITS, S], BF16)
        small = sb("small_sb", [128, NSMALL], F32)
        deriv = sb("deriv_sb", [128, NDERIV], F32)
        cst = sb("cst_sb", [128, NCONST], BF16)
        recb = sb("recb", [64, 512], F32)
        tick = sb("tick", [128, 4], F32)
        Sst = sb("Sst", [128, 128], F32)
        Sbb = sb("Sbb", [128, 2, 128], BF16)
        ps = es.enter_context(nc.psum_tensor("ps", [128, 8, 512], F32))
        psf = ps[:, :, :].rearrange("p b c -> p (b c)")

        PE = Eng(nc, es, "pe", nc.tensor, skip_self=True)
        ACT = Eng(nc, es, "act", nc.scalar)
        DVE = Eng(nc, es, "dve", nc.vector)
        POOL = Eng(nc, es, "pool", nc.gpsimd)
        SP = Eng(nc, es, "sp", nc.sync)
        ring_sems = [SemObj(nc, es, f"ring{i}") for i in range(RING_SLOTS)]
        sem_xc = [SemObj(nc, es, f"xload{c}") for c in range(NKC)]
        sem_misc = SemObj(nc, es, "misc")
        sem_cst = SemObj(nc, es, "cstload")
        sem_out = SemObj(nc, es, "outst")
        sem_rows = [SemObj(nc, es, f"rows{i}") for i in range(8)]
        sem_chlw = SemObj(nc, es, "chlw")

        x_res = [Res(f"x{c}") for c in range(NKC)]
        h_res = [Res(f"h{c}") for c in range(NKC)]
        u_res = [Res(f"u{i}") for i in range(NUNITS)]
        bank = [Res(f"bank{i}") for i in range(8)]
        ring_res = [Res(f"ring{i}") for i in range(RING_SLOTS)]
        small_res = Res("small")
        deriv_res = Res("deriv")
        cst_res = Res("cst")
        S_res = Res("S")
        recb_res = Res("recb")
        chl_dram_res = Res("chl_dram")
        Sb_res = [Res("Sb0"), Res("Sb1")]

        def ubuf(u0, n, dt=BF16, parts=128, shape=None):
            ap = U[0:parts, u0:u0 + n, :].rearrange("p a b -> p (a b)")
            if dt == F32:
                ap = ap.bitcast(F32)
            if shape is not None:
                ap = ap.rearrange(shape[0], **shape[1])
            return Buf(ap, u_res[u0:u0 + n])

        ident = cst[:, K_IDENT:K_IDENT + 128]
        onesb = cst[:, K_ONES:K_ONES + 128]
        maskneg = cst[:, K_MASKNEG:K_MASKNEG + 128]
        hmask = cst[:, K_HMASK:K_HMASK + 512]
        scanmask = cst[:, K_SCAN:K_SCAN + S]

        def v4(ap):
            return ap.rearrange("p (a b) -> p a b", a=4)

        class WStream:
            def __init__(self):
                self.plan = []
                self.i_issue = 0
                self.i_get = 0

            def _issue(self, i):
                key, src, a, b = self.plan[i]
                slot = i % RING_SLOTS
                dst = ring[:, slot, 0:a * b].rearrange("p (a b) -> p a b", a=a)
                T.dma(POOL, ring_sems[slot],
                      lambda: nc.gpsimd.dma_start(out=dst, in_=src),
                      writes=[ring_res[slot]])

            def get(self, key, src, a, b, hold=0):
                if T.plan:
                    self.plan.append((key, src, a, b))
                    return Buf(ring[:, 0, 0:a * b].rearrange("p (a b) -> p a b", a=a), [ring_res[0]])
                i = self.i_get
                self.i_get += 1
                assert self.plan[i][0] == key, (self.plan[i][0], key)
                while self.i_issue < len(self.plan) and self.i_issue <= i - 1 - hold + RING_SLOTS:
                    self._issue(self.i_issue)
                    self.i_issue += 1
                slot = i % RING_SLOTS
                return Buf(ring[:, slot, 0:a * b].rearrange("p (a b) -> p a b", a=a), [ring_res[slot]])

        W = WStream()

        def proj_fm(w, c0, m, banks, m_out=None):
            for kc in range(NKC):
                def fn():
                    inst = None
                    for tt in range(4):
                        inst = nc.tensor.matmul(ps[0:m, banks[tt], :], lhsT=w.ap[:, kc, c0:c0 + m],
                                                rhs=hT[:, kc, tt * 512:(tt + 1) * 512],
                                                start=(kc == 0), stop=(kc == NKC - 1))
                    return inst
                T.op(PE, fn, reads=flat_res(w, h_res[kc]), writes=[bank[b] for b in banks])

        def rmsnorm(gcol, sq, rstd):
            for c in range(NKC):
                sqb = sq[c % 2]
                T.op(ACT, lambda: nc.scalar.activation(out=sqb.ap, in_=xT[:, c, :], func=AF.Square),
                     reads=[x_res[c]], writes=sqb.res)

                def fn():
                    inst = None
                    for tt in range(4):
                        inst = nc.tensor.matmul(ps[:, tt, :], lhsT=onesb, rhs=sqb.ap[:, tt * 512:(tt + 1) * 512],
                                                start=(c == 0), stop=(c == NKC - 1))
                    return inst
                T.op(PE, fn, reads=flat_res(sqb, cst_res), writes=bank[0:4])
            T.op(ACT, lambda: nc.scalar.activation(out=v4(rstd.ap), in_=ps[:, 0:4, :], func=AF.Ln,
                                                   scale=1.0 / D, bias=eps_ap),
                 reads=flat_res(bank[0:4], deriv_res), writes=rstd.res)
            T.op(ACT, lambda: nc.scalar.activation(out=rstd.ap, in_=rstd.ap, func=AF.Exp, scale=-0.5),
                 reads=rstd.res, writes=rstd.res)
            for c in range(NKC):
                T.op(DVE, lambda: nc.vector.scalar_tensor_tensor(
                    out=hT[:, c, :], in0=xT[:, c, :], scalar=small[:, gcol + c:gcol + c + 1],
                    in1=rstd.ap, op0=ALU.mult, op1=ALU.mult),
                    reads=flat_res(x_res[c], rstd, small_res), writes=[h_res[c]])

        def out_accum(key, src_fn, nk, rhs_list):
            for dcp in range(4):
                w = W.get((key, dcp), src_fn(dcp), nk, 256)
                for dcl in range(2):
                    dc = dcp * 2 + dcl
                    b0 = 4 * dcl

                    def fn():
                        inst = None
                        for j in range(nk):
                            for tt in range(4):
                                inst = nc.tensor.matmul(ps[:, b0 + tt, :], lhsT=w.ap[:, j, dcl * 128:(dcl + 1) * 128],
                                                        rhs=rhs_list[j].ap[:, tt * 512:(tt + 1) * 512],
                                                        start=(j == 0), stop=(j == nk - 1))
                        return inst
                    T.op(PE, fn, reads=flat_res(w, rhs_list), writes=bank[b0:b0 + 4])
                    T.op(DVE, lambda: nc.vector.tensor_tensor(out=v4(xT[:, dc, :]), in0=v4(xT[:, dc, :]),
                                                              in1=ps[:, b0:b0 + 4, :], op=ALU.add),
                         reads=flat_res(bank[b0:b0 + 4], x_res[dc]), writes=[x_res[dc]])

        def ffn(gl, fi):
            sq = [ubuf(13, 1), ubuf(14, 1)]
            rstd = ubuf(15, 2, F32)
            rmsnorm(C_GFFN + gl * 8, sq, rstd)
            actT = [ubuf(j, 1) for j in range(11)]
            stmp = [ubuf(11, 1), ubuf(12, 1)]
            for half in range(2):
                for jj in range(11):
                    hc = half * 11 + jj
                    src = ffn_in_d[fi].rearrange("(kc p) h c -> p kc h c", p=128)[:, :, hc, :]
                    w = W.get(("ffn_in", gl, hc), src, NKC, 256)
                    proj_fm(w, 0, 128, [0, 1, 2, 3])
                    proj_fm(w, 128, 128, [4, 5, 6, 7])
                    st = stmp[jj % 2]
                    T.op(ACT, lambda: nc.scalar.activation(out=v4(st.ap), in_=ps[:, 0:4, :], func=AF.Silu),
                         reads=bank[0:4], writes=st.res)
                    T.op(DVE, lambda: nc.vector.tensor_tensor(out=v4(actT[jj].ap), in0=v4(st.ap),
                                                              in1=ps[:, 4:8, :], op=ALU.mult),
                         reads=flat_res(st, bank[4:8]), writes=actT[jj].res)

                def src_fn(dcp, half=half):
                    r0 = half * 1408
                    return ffn_out_d[fi][r0:r0 + 1408, dcp * 256:(dcp + 1) * 256].rearrange(
                        "(j p) c -> p j c", p=128)
                out_accum(("ffn_out", gl, half), src_fn, 11, actT)

        def fox_layer(gl, ai):
            gj = gl // 2
            sq = [ubuf(13, 1), ubuf(14, 1)]
            rstd = ubuf(0, 2, F32)
            rmsnorm(C_GMIX + gl * 8, sq, rstd)
            bout = [ubuf(13 + j, 1) for j in range(4)]
            aout = [ubuf(17 + j, 1) for j in range(4)]

            F0 = ubuf(0, 2, F32, parts=8)
            F1 = ubuf(2, 2, F32, parts=8)
            ones1 = ubuf(4, 1, BF16, parts=8)
            chl = ubuf(5, 3, BF16, parts=8, shape=("p (a b) -> p a b", dict(a=3)))
            src = ab_f_d[ai].rearrange("(kc p) c -> p kc c", p=128)
            w = W.get(("ab_f", gl), src, NKC, 8)
            proj_fm(w, 0, 8, [0, 1, 2, 3])
            nfb = deriv[0:8, DV_NFB + gj:DV_NFB + gj + 1]
            T.op(ACT, lambda: nc.scalar.activation(out=v4(F0.ap), in_=ps[0:8, 0:4, :], func=AF.Exp,
                                                   scale=-1.0, bias=nfb),
                 reads=flat_res(bank[0:4], deriv_res), writes=F0.res)
            T.op(ACT, lambda: nc.scalar.activation(out=F0.ap, in_=F0.ap, func=AF.Ln, scale=1.0, bias=one_ap[0:8, :]),
                 reads=flat_res(F0, deriv_res), writes=F0.res)
            T.op(DVE, lambda: nc.vector.memset(ones1.ap, 1.0), writes=ones1.res)
            T.op(DVE, lambda: nc.vector.tensor_tensor_scan(out=F1.ap, data0=ones1.ap, data1=F0.ap, initial=0.0,
                                                           op0=ALU.mult, op1=ALU.subtract),
                 reads=flat_res(ones1, F0), writes=F1.res)
            T.op(DVE, lambda: nc.vector.tensor_copy(out=chl.ap[:, 0, :], in_=F1.ap), reads=F1.res, writes=chl.res)
            T.op(DVE, lambda: nc.vector.tensor_tensor(out=F0.ap, in0=F1.ap, in1=chl.ap[:, 0, :], op=ALU.subtract),
                 reads=flat_res(F1, chl), writes=F0.res)
            T.op(DVE, lambda: nc.vector.tensor_copy(out=chl.ap[:, 1, :], in_=F0.ap), reads=F0.res, writes=chl.res)
            T.op(DVE, lambda: nc.vector.tensor_tensor(out=F1.ap, in0=F0.ap, in1=chl.ap[:, 1, :], op=ALU.subtract),
                 reads=flat_res(F0, chl), writes=F1.res)
            T.op(DVE, lambda: nc.vector.tensor_copy(out=chl.ap[:, 2, :], in_=F1.ap), reads=F1.res, writes=chl.res)
            T.dma(SP, sem_chlw, lambda: nc.sync.dma_start(out=chl_d[:, :, :], in_=chl.ap), reads=chl.res,
                  writes=[chl_dram_res])

            T0 = ubuf(0, 2, F32)
            Zb = Buf(U[:, 2:5, :].rearrange("p a b -> p (a b)").bitcast(F32)[:, 0:S + 2], u_res[2:5])
            Yb = ubuf(5, 2, F32)
            T.op(DVE, lambda: nc.vector.memset(Zb.ap[:, 0:2], 0.0), writes=Zb.res)
            for j in range(4):
                gA = [0, 1, 2, 3] if j % 2 == 0 else [4, 5, 6, 7]
                gB = [4, 5, 6, 7] if j % 2 == 0 else [0, 1, 2, 3]
                a0, b0 = gA[0], gB[0]
                src = ab_conv_d[ai].rearrange("(kc p) j c -> p kc j c", p=128)[:, :, j, 0:256]
                wcx = W.get(("ab_conv_cx", gl, j), src, NKC, 256)
                proj_fm(wcx, 0, 128, gA)
                proj_fm(wcx, 128, 128, gB)
                T.op(ACT, lambda: nc.scalar.copy(out=v4(T0.ap), in_=ps[:, a0:a0 + 4, :]), reads=bank[a0:a0 + 4],
                     writes=T0.res)
                T.op(DVE, lambda: nc.vector.tensor_tensor(out=v4(Zb.ap[:, 2:S + 2]), in0=v4(T0.ap),
                                                          in1=ps[:, b0:b0 + 4, :], op=ALU.mult),
                     reads=flat_res(T0, bank[b0:b0 + 4]), writes=Zb.res)
                src = ab_conv_d[ai].rearrange("(kc p) j c -> p kc j c", p=128)[:, :, j, 256:384]
                wb = W.get(("ab_conv_b", gl, j), src, NKC, 128)
                proj_fm(wb, 0, 128, gA)
                cw = lambda tap: small[:, C_CONVW + (gj * 3 + tap) * 4 + j:C_CONVW + (gj * 3 + tap) * 4 + j + 1]
                T.op(DVE, lambda: nc.vector.tensor_scalar(out=Yb.ap, in0=Zb.ap[:, 0:S], scalar1=cw(0), scalar2=None,
                                                          op0=ALU.mult),
                     reads=flat_res(Zb, small_res), writes=Yb.res)
                T.op(DVE, lambda: nc.vector.scalar_tensor_tensor(out=Yb.ap, in0=Zb.ap[:, 1:S + 1], scalar=cw(1),
                                                                 in1=Yb.ap, op0=ALU.mult, op1=ALU.add),
                     reads=flat_res(Zb, Yb, small_res), writes=Yb.res)
                T.op(DVE, lambda: nc.vector.scalar_tensor_tensor(out=Yb.ap, in0=Zb.ap[:, 2:S + 2], scalar=cw(2),
                                                                 in1=Yb.ap, op0=ALU.mult, op1=ALU.add),
                     reads=flat_res(Zb, Yb, small_res), writes=Yb.res)
                T.op(DVE, lambda: nc.vector.tensor_tensor(out=v4(bout[j].ap), in0=v4(Yb.ap), in1=ps[:, a0:a0 + 4, :],
                                                          op=ALU.mult),
                     reads=flat_res(Yb, bank[a0:a0 + 4]), writes=bout[j].res)

            qaug = [ubuf(i, 1) for i in range(4)]
            kaug = [ubuf(4 + i, 1) for i in range(4)]
            vaug = [ubuf(8 + i, 1, shape=("p (a b) -> p a b", dict(a=16))) for i in range(4)]
            pt_res = [Res(f"pt{i}") for i in range(4)]
            PT = [Buf(U[:, 12, i * 512:(i + 1) * 512], [pt_res[i]]) for i in range(4)]
            T.op(DVE, lambda: nc.vector.memset(tick[:, 0:1], 0.0), reads=[u_res[12]], writes=pt_res + [u_res[12]])
            rb = Buf(recb[:, :], [recb_res])
            for i in range(4):
                T.op(DVE, lambda: nc.vector.memset(qaug[i].ap[64:70, :], -1.0), writes=qaug[i].res)
                T.op(DVE, lambda: nc.vector.memset(kaug[i].ap[64:70, :], 1.0), writes=kaug[i].res)
                T.op(DVE, lambda: nc.vector.memset(vaug[i].ap[:, :, 64:128], 1.0), writes=vaug[i].res)
            sbanks = [0, 1, 2]

            def pair_fillers(g):
                s2 = (g % 2) * 2
                src = ab_att_d[ai].rearrange("(kc p) g c -> p kc g c", p=128)[:, :, g, :]
                watt = W.get(("ab_att", gl, g), src, NKC, 384)
                fl = []
                for which, c0, dst in ((0, 0, qaug), (1, 128, kaug)):
                    for half in range(2):
                        for kc in range(NKC):
                            def fn(kc=kc, half=half, c0=c0):
                                def mm():
                                    inst = None
                                    for t2 in range(2):
                                        tt = half * 2 + t2
                                        inst = nc.tensor.matmul(ps[:, 5 + t2, :], lhsT=watt.ap[:, kc, c0:c0 + 128],
                                                                rhs=hT[:, kc, tt * 512:(tt + 1) * 512],
                                                                start=(kc == 0), stop=(kc == NKC - 1))
                                    return inst
                                T.op(PE, mm, reads=flat_res(watt, h_res[kc]), writes=bank[5:7])
                            fl.append(fn)
                        for hh in range(2):
                            def fe(hh=hh, half=half, which=which, dst=dst):
                                o = dst[s2 + hh].ap[0:64, half * 1024:(half + 1) * 1024].rearrange("p (a b) -> p a b", a=2)
                                i_ = ps[hh * 64:hh * 64 + 64, 5:7, :]
                                if which == 0:
                                    T.op(DVE, lambda: nc.vector.tensor_scalar(out=o, in0=i_, scalar1=0.125, scalar2=None,
                                                                              op0=ALU.mult),
                                         reads=bank[5:7], writes=dst[s2 + hh].res)
                                else:
                                    T.op(DVE, lambda: nc.vector.tensor_copy(out=o, in_=i_),
                                         reads=bank[5:7], writes=dst[s2 + hh].res)
                            fl.append(fe)
                for qd in range(4):
                    for t4 in range(4):
                        def fv(qd=qd, t4=t4):
                            tt = qd * 4 + t4

                            def mm():
                                inst = None
                                for kc in range(NKC):
                                    inst = nc.tensor.matmul(ps[:, 7, t4 * 128:(t4 + 1) * 128],
                                                            lhsT=hT[:, kc, tt * 128:(tt + 1) * 128],
                                                            rhs=watt.ap[:, kc, 256:384],
                                                            start=(kc == 0), stop=(kc == NKC - 1))
                                return inst
                            T.op(PE, mm, reads=flat_res(watt, h_res), writes=[bank[7]])
                        fl.append(fv)
                    for hh in range(2):
                        def fve(qd=qd, hh=hh):
                            o = vaug[s2 + hh].ap[:, qd * 4:(qd + 1) * 4, 0:64]
                            i_ = ps[:, 7, :].rearrange("p (a b) -> p a b", a=4)[:, :, hh * 64:(hh + 1) * 64]
                            T.op(DVE, lambda: nc.vector.tensor_copy(out=o, in_=i_), reads=[bank[7]],
                                 writes=vaug[s2 + hh].res)
                        fl.append(fve)
                for hh in range(2):
                    def fr(hh=hh):
                        h = 2 * g + hh
                        T.dma(SP, sem_rows[(s2 + hh) * 2],
                              lambda: nc.sync.dma_start(out=qaug[s2 + hh].ap[64:67, :], in_=chl_d[h, :, :]),
                              reads=[chl_dram_res], writes=qaug[s2 + hh].res)
                        T.dma(SP, sem_rows[(s2 + hh) * 2 + 1],
                              lambda: nc.sync.dma_start(out=kaug[s2 + hh].ap[67:70, :], in_=chl_d[h, :, :]),
                              reads=[chl_dram_res], writes=kaug[s2 + hh].res)
                    fl.append(fr)
                return fl

            steps = []
            for qt in range(4):
                for j in range(4 * qt + 4):
                    r = j - 4 * qt
                    steps.append((qt, j, 128 * r if r > 0 else 0, r >= 0))

            for f in pair_fillers(0):
                f()
            for g in range(4):
                fillers = pair_fillers(g + 1) if g < 3 else []
                for hh in range(2):
                    h = 2 * g + hh
                    i = (g % 2) * 2 + hh
                    deferred = []

                    def emit_s(si):
                        qt, j, off, diag = steps[si]
                        sbk = sbanks[si % 3]

                        def fn():
                            inst = nc.tensor.matmul(ps[:, sbk, off:512], lhsT=kaug[i].ap[0:70, j * 128:(j + 1) * 128],
                                                    rhs=qaug[i].ap[0:70, qt * 512 + off:(qt + 1) * 512],
                                                    start=True, stop=(not diag))
                            if diag:
                                inst = nc.tensor.matmul(ps[:, sbk, off:off + 128], lhsT=ident, rhs=maskneg,
                                                        start=False, stop=True)
                            return inst
                        T.op(PE, fn, reads=flat_res(kaug[i], qaug[i], cst_res), writes=[bank[sbk]])

                    def emit_norm(qt):
                        ob = 3 + qt % 2
                        T.op(ACT, lambda: nc.scalar.activation(out=rb.ap, in_=ps[64:128, ob, :], func=AF.Ln),
                             reads=[bank[ob]], writes=rb.res)
                        T.op(ACT, lambda: nc.scalar.activation(out=rb.ap, in_=rb.ap, func=AF.Exp, scale=-1.0),
                             reads=rb.res, writes=rb.res)
                        T.op(DVE, lambda: nc.vector.tensor_tensor(
                            out=aout[h // 2].ap[(h % 2) * 64:(h % 2) * 64 + 64, qt * 512:(qt + 1) * 512],
                            in0=ps[0:64, ob, :], in1=rb.ap, op=ALU.mult),
                            reads=flat_res(bank[ob], rb), writes=aout[h // 2].res)

                    emit_s(0)
                    emit_s(1)
                    for si, (qt, j, off, diag) in enumerate(steps):
                        if si + 2 < len(steps):
                            emit_s(si + 2)
                        sbk = sbanks[si % 3]
                        pt = PT[si % 4]
                        ob = 3 + qt % 2
                        T.op(ACT, lambda: nc.scalar.activation(out=pt.ap[:, off:512], in_=ps[:, sbk, off:512],
                                                               func=AF.Exp),
                             reads=[bank[sbk]], writes=pt.res)
                        if fillers:
                            fillers.pop(0)()
                        T.op(PE, lambda: nc.tensor.matmul(ps[:, ob, off:512], lhsT=vaug[i].ap[:, j, :],
                                                          rhs=pt.ap[:, off:512], start=(j == 0), stop=(j == 4 * qt + 3)),
                             reads=flat_res(pt, vaug[i]), writes=[bank[ob]])
                        deferred = [(d - 1, q) for d, q in deferred]
                        while deferred and deferred[0][0] <= 0:
                            emit_norm(deferred.pop(0)[1])
                        if j == 4 * qt + 3:
                            deferred.append((2, qt))
                    for _, q in deferred:
                        emit_norm(q)
                for f in fillers:
                    f()

            T.op(DVE, lambda: nc.vector.memset(tick[:, 0:1], 0.0), reads=pt_res, writes=pt_res + [u_res[12]])

            def src_fn(dcp):
                return ab_out_d[ai][:, dcp * 256:(dcp + 1) * 256].rearrange("(j p) c -> p j c", p=128)
            out_accum(("ab_out", gl), src_fn, 8, aout + bout)

        def hgrn_layer(gl, ci):
            sq = [ubuf(8, 1), ubuf(9, 1)]
            rstd = ubuf(0, 2, F32)
            rmsnorm(C_GMIX + gl * 8, sq, rstd)
            Ab = ubuf(0, 2, F32)
            Bb = ubuf(2, 2, F32)
            Cb = ubuf(4, 2, F32)
            Db = ubuf(6, 2, F32)
            KDT = ubuf(4, 1)
            ATm = Buf(U[:, 5, 0:1024], [u_res[5]])
            KD = ubuf(6, 1, shape=("p (a b) -> p a b", dict(a=16)))
            QT = ubuf(8, 1)
            KT = ubuf(9, 1)
            Vb = ubuf(10, 1, shape=("p (a b) -> p a b", dict(a=16)))
            Gb = ubuf(11, 1)
            o2 = ubuf(10, 1)
            Tb = ubuf(8, 2, F32)
            OT = [ubuf(12 + h, 1) for h in range(8)]
            E3 = Bb.ap.rearrange("p (c t) -> p c t", t=64)
            psb45 = psf[:, 2048:3072].bitcast(BF16)
            Qraw = ubuf(20, 1)

            def slices(w, c0, evac):
                fl = []
                for half in range(2):
                    for kc in range(NKC):
                        def fn(kc=kc, half=half):
                            def mm():
                                inst = None
                                for t2 in range(2):
                                    tt = half * 2 + t2
                                    inst = nc.tensor.matmul(ps[:, 6 + t2, :], lhsT=w.ap[:, kc, c0:c0 + 128],
                                                            rhs=hT[:, kc, tt * 512:(tt + 1) * 512],
                                                            start=(kc == 0), stop=(kc == NKC - 1))
                                return inst
                            T.op(PE, mm, reads=flat_res(w, h_res[kc]), writes=bank[6:8])
                        fl.append(fn)
                    fl.append(lambda half=half: evac(half))
                return fl

            def h2(ap, half):
                return ap[:, half * 1024:(half + 1) * 1024].rearrange("p (a b) -> p a b", a=2)

            def pre_project(hn_):
                src = c_in_d[ci].rearrange("(kc p) h c -> p kc h c", p=128)[:, :, hn_, 0:256]
                wA = W.get(("c_in_A", gl, hn_), src, NKC, 256, hold=(1 if hn_ > 0 else 0))
                fl = slices(wA, 128, lambda half: T.op(
                    ACT, lambda: nc.scalar.activation(out=h2(Ab.ap, half), in_=ps[:, 6:8, :], func=AF.Sigmoid),
                    reads=bank[6:8], writes=Ab.res))
                fl += slices(wA, 0, lambda half: T.op(
                    DVE, lambda: nc.vector.tensor_copy(out=h2(Qraw.ap, half), in_=ps[:, 6:8, :]),
                    reads=bank[6:8], writes=Qraw.res))
                return fl

            def prep1(hh):
                gj_ = gl // 2
                lb_ = deriv[:, DV_LB + gj_ * 8 + hh:DV_LB + gj_ * 8 + hh + 1]
                oml_ = deriv[:, DV_OML + gj_ * 8 + hh:DV_OML + gj_ * 8 + hh + 1]
                T.op(ACT, lambda: nc.scalar.activation(out=Bb.ap, in_=Ab.ap, func=AF.Ln, scale=oml_, bias=lb_),
                     reads=flat_res(Ab, deriv_res), writes=Bb.res)
                T.op(DVE, lambda: nc.vector.tensor_tensor_scan(out=Cb.ap, data0=scanmask, data1=Bb.ap, initial=0.0,
                                                               op0=ALU.mult, op1=ALU.add),
                     reads=flat_res(Bb, cst_res), writes=Cb.res)
                noml_ = deriv[:, DV_NOML + gj_ * 8 + hh:DV_NOML + gj_ * 8 + hh + 1]
                T.op(DVE, lambda: nc.vector.tensor_scalar(out=Ab.ap, in0=Ab.ap, scalar1=noml_, scalar2=oml_,
                                                          op0=ALU.mult, op1=ALU.add),
                     reads=flat_res(Ab, deriv_res), writes=Ab.res)

            for h in range(8):
                gj = gl // 2
                lb = deriv[:, DV_LB + gj * 8 + h:DV_LB + gj * 8 + h + 1]
                oml = deriv[:, DV_OML + gj * 8 + h:DV_OML + gj * 8 + h + 1]
                noml = deriv[:, DV_NOML + gj * 8 + h:DV_NOML + gj * 8 + h + 1]
                hn = small[:, C_HNORM + gj * 8 + h:C_HNORM + gj * 8 + h + 1]
                if h == 0:
                    for f in pre_project(0):
                        f()
                if h == 0:
                    prep1(0)
                T.op(ACT, lambda: nc.scalar.activation(out=Bb.ap, in_=Cb.ap, func=AF.Exp),
                     reads=Cb.res, writes=Bb.res)
                T.op(ACT, lambda: nc.scalar.activation(out=Db.ap, in_=Cb.ap, func=AF.Exp, scale=-1.0),
                     reads=Cb.res, writes=Db.res)
                T.op(DVE, lambda: nc.vector.tensor_tensor(out=QT.ap, in0=Qraw.ap, in1=Bb.ap, op=ALU.mult),
                     reads=flat_res(Qraw, Bb), writes=QT.res)
                T.op(DVE, lambda: nc.vector.tensor_tensor(out=KT.ap, in0=Ab.ap, in1=Db.ap, op=ALU.mult),
                     reads=flat_res(Ab, Db), writes=KT.res)
                T.op(DVE, lambda: nc.vector.tensor_tensor(
                    out=KDT.ap.rearrange("p (c t) -> p c t", t=64), in0=KT.ap.rearrange("p (c t) -> p c t", t=64),
                    in1=E3[:, :, 63:64].broadcast_to([128, 32, 64]), op=ALU.mult),
                    reads=flat_res(KT, Bb), writes=KDT.res)
                src = c_in_d[ci].rearrange("(kc p) h c -> p kc h c", p=128)[:, :, h, 256:512]
                wB = W.get(("c_in_B", gl, h), src, NKC, 256)

                def fnv():
                    inst = None
                    for tt in range(16):
                        for kc in range(NKC):
                            inst = nc.tensor.matmul(ps[:, 4 + tt // 4, (tt % 4) * 128:(tt % 4) * 128 + 128],
                                                    lhsT=hT[:, kc, tt * 128:(tt + 1) * 128],
                                                    rhs=wB.ap[:, kc, 0:128],
                                                    start=(kc == 0), stop=(kc == NKC - 1))
                    return inst
                T.op(PE, fnv, reads=flat_res(wB, h_res), writes=bank[4:8])
                T.op(ACT, lambda: nc.scalar.copy(out=Vb.ap.rearrange("p (a c) e -> p a (c e)", a=4), in_=ps[:, 4:8, :]),
                     reads=bank[4:8], writes=Vb.res)

                def fnt():
                    inst = None
                    for tt in range(16):
                        inst = nc.tensor.transpose(psb45[:, tt * 128:(tt + 1) * 128],
                                                   KDT.ap[:, tt * 128:(tt + 1) * 128], ident)
                    return inst
                T.op(PE, fnt, reads=flat_res(KDT, cst_res), writes=bank[4:6])
                T.op(DVE, lambda: nc.vector.tensor_copy(out=KD.ap.rearrange("p a b -> p (a b)"), in_=psb45),
                     reads=bank[4:6], writes=KD.res)

                def fna():
                    inst = None
                    for c in range(32):
                        r0 = (c % 2) * 64
                        inst = nc.tensor.matmul(ps[r0:r0 + 64, 6 + c // 16, ((c // 2) % 8) * 64:((c // 2) % 8) * 64 + 64],
                                                lhsT=KT.ap[:, c * 64:(c + 1) * 64], rhs=QT.ap[:, c * 64:(c + 1) * 64],
                                                start=True, stop=True)
                    return inst
                T.op(PE, fna, reads=flat_res(KT, QT), writes=bank[6:8])
                for a in range(2):
                    T.op(DVE, lambda: nc.vector.tensor_tensor(out=ATm.ap[:, a * 512:(a + 1) * 512], in0=ps[:, 6 + a, :],
                                                              in1=hmask, op=ALU.mult),
                         reads=flat_res(bank[6 + a], cst_res), writes=ATm.res)
                T.op(DVE, lambda: nc.vector.memset(Sst[:, :], 0.0), writes=[S_res])
                fillers = slices(wB, 128, lambda half: T.op(
                    ACT, lambda: nc.scalar.activation(out=h2(Gb.ap, half), in_=ps[:, 6:8, :], func=AF.Silu),
                    reads=bank[6:8], writes=Gb.res))
                if h < 7:
                    fillers += pre_project(h + 1)
                for c in range(32):
                    tt = c // 2
                    r0 = (c % 2) * 64
                    ocol = (c % 8) * 64

                    def fno():
                        inst = nc.tensor.matmul(ps[:, c // 8, ocol:ocol + 64], lhsT=Vb.ap[r0:r0 + 64, tt, :],
                                                rhs=ATm.ap[r0:r0 + 64, tt * 64:(tt + 1) * 64],
                                                start=True, stop=(c == 0))
                        if c > 0:
                            inst = nc.tensor.matmul(ps[:, c // 8, ocol:ocol + 64], lhsT=Sbb[:, (c - 1) % 2, :],
                                                    rhs=QT.ap[:, c * 64:(c + 1) * 64], start=False, stop=True)
                        return inst
                    rd = flat_res(Vb, ATm, QT) + ([Sb_res[(c - 1) % 2]] if c > 0 else [])
                    T.op(PE, fno, reads=rd, writes=[bank[c // 8]])
                    if c < 31:
                        db = 4
                        T.op(PE, lambda: nc.tensor.matmul(ps[:, db, 0:128], lhsT=KD.ap[r0:r0 + 64, tt, :],
                                                          rhs=Vb.ap[r0:r0 + 64, tt, :], start=True, stop=True),
                             reads=flat_res(KD, Vb), writes=[bank[db]])
                        T.op(DVE, lambda: nc.vector.scalar_tensor_tensor(
                            out=Sst[:, :], in0=Sst[:, :], scalar=Bb.ap[:, c * 64 + 63:c * 64 + 64], in1=ps[:, db, 0:128],
                            op0=ALU.mult, op1=ALU.add),
                            reads=flat_res(S_res, Bb, bank[db]), writes=[S_res])
                        T.op(ACT, lambda: nc.scalar.copy(out=Sbb[:, c % 2, :], in_=Sst[:, :]),
                             reads=[S_res], writes=[Sb_res[c % 2]])
                    for _ in range(2):
                        if fillers:
                            fillers.pop(0)()
                for f in fillers:
                    f()
                if h < 7:
                    prep1(h + 1)
                T.op(ACT, lambda: nc.scalar.activation(out=v4(o2.ap), in_=ps[:, 0:4, :], func=AF.Square),
                     reads=bank[0:4], writes=o2.res)

                def fns():
                    inst = None
                    for tt in range(4):
                        inst = nc.tensor.matmul(ps[:, 4 + tt, :], lhsT=onesb, rhs=o2.ap[:, tt * 512:(tt + 1) * 512],
                                                start=True, stop=True)
                    return inst
                T.op(PE, fns, reads=flat_res(o2, cst_res), writes=bank[4:8])
                T.op(ACT, lambda: nc.scalar.activation(out=v4(Db.ap), in_=ps[:, 4:8, :], func=AF.Ln,
                                                       scale=1.0 / 128, bias=eps_ap),
                     reads=flat_res(bank[4:8], deriv_res), writes=Db.res)
                T.op(ACT, lambda: nc.scalar.activation(out=Db.ap, in_=Db.ap, func=AF.Exp, scale=-0.5),
                     reads=Db.res, writes=Db.res)
                T.op(DVE, lambda: nc.vector.scalar_tensor_tensor(out=v4(Tb.ap), in0=ps[:, 0:4, :], scalar=hn,
                                                                 in1=v4(Db.ap), op0=ALU.mult, op1=ALU.mult),
                     reads=flat_res(bank[0:4], Db, small_res), writes=Tb.res)
                T.op(DVE, lambda: nc.vector.tensor_tensor(out=OT[h].ap, in0=Tb.ap, in1=Gb.ap, op=ALU.mult),
                     reads=flat_res(Tb, Gb), writes=OT[h].res)

            def src_fn(dcp):
                return c_out_d[ci][:, dcp * 256:(dcp + 1) * 256].rearrange("(j p) c -> p j c", p=128)
            out_accum(("c_out", gl), src_fn, 8, OT)

        eps_ap = deriv[:, NDERIV - 1:NDERIV]
        one_ap = deriv[:, NDERIV - 2:NDERIV - 1]

        def setup():
            T.dma(SP, sem_misc, lambda: nc.sync.dma_start(out=small[:, :], in_=small_d[:, :]), writes=[small_res])
            T.dma(POOL, sem_cst, lambda: nc.gpsimd.dma_start(out=cst[:, :], in_=const_d[:, :]), writes=[cst_res])
            for c in range(NKC):
                T.dma(SP, sem_xc[c], lambda: nc.sync.dma_start(out=xT[:, c, :], in_=xT_d[c * 128:(c + 1) * 128, :]),
                      writes=[x_res[c]])
            T.op(DVE, lambda: nc.vector.memset(deriv[:, :], 0.0), writes=[deriv_res])
            T.op(DVE, lambda: nc.vector.memset(deriv[:, NDERIV - 1:NDERIV], EPS), writes=[deriv_res])
            T.op(DVE, lambda: nc.vector.memset(deriv[:, NDERIV - 2:NDERIV - 1], 1.0), writes=[deriv_res])
            T.op(DVE, lambda: nc.vector.tensor_tensor(out=deriv[:, DV_LB + 8:DV_LB + 16], in0=small[:, C_CLB + 8:C_CLB + 16],
                                                      in1=small[:, C_CLB:C_CLB + 8], op=ALU.subtract),
                 reads=[small_res, deriv_res], writes=[deriv_res])
            T.op(ACT, lambda: nc.scalar.activation(out=deriv[:, DV_LB + 8:DV_LB + 16], in_=deriv[:, DV_LB + 8:DV_LB + 16],
                                                   func=AF.Sigmoid),
                 reads=[deriv_res], writes=[deriv_res])
            T.op(DVE, lambda: nc.vector.tensor_scalar(out=deriv[:, DV_OML:DV_OML + 16], in0=deriv[:, DV_LB:DV_LB + 16],
                                                      scalar1=-1.0, scalar2=1.0, op0=ALU.mult, op1=ALU.add),
                 reads=[deriv_res], writes=[deriv_res])
            T.op(DVE, lambda: nc.vector.tensor_scalar(out=deriv[:, DV_NOML:DV_NOML + 16], in0=deriv[:, DV_LB:DV_LB + 16],
                                                      scalar1=1.0, scalar2=-1.0, op0=ALU.mult, op1=ALU.add),
                 reads=[deriv_res], writes=[deriv_res])
            T.op(DVE, lambda: nc.vector.tensor_scalar(out=deriv[:, DV_NFB:DV_NFB + 2], in0=small[:, C_FBIAS:C_FBIAS + 2],
                                                      scalar1=-1.0, scalar2=None, op0=ALU.mult),
                 reads=[small_res, deriv_res], writes=[deriv_res])

        def finish():
            sq = [ubuf(13, 1), ubuf(14, 1)]
            rstd = ubuf(15, 2, F32)
            if final:
                for c in range(NKC):
                    sqb = sq[c % 2]
                    T.op(ACT, lambda: nc.scalar.activation(out=sqb.ap, in_=xT[:, c, :], func=AF.Square),
                         reads=[x_res[c]], writes=sqb.res)

                    def fn():
                        inst = None
                        for tt in range(4):
                            inst = nc.tensor.matmul(ps[:, tt, :], lhsT=onesb, rhs=sqb.ap[:, tt * 512:(tt + 1) * 512],
                                                    start=(c == 0), stop=(c == NKC - 1))
                        return inst
                    T.op(PE, fn, reads=flat_res(sqb, cst_res), writes=bank[0:4])
                T.op(ACT, lambda: nc.scalar.activation(out=v4(rstd.ap), in_=ps[:, 0:4, :], func=AF.Ln,
                                                       scale=1.0 / D, bias=eps_ap),
                     reads=flat_res(bank[0:4], deriv_res), writes=rstd.res)
                T.op(ACT, lambda: nc.scalar.activation(out=rstd.ap, in_=rstd.ap, func=AF.Exp, scale=-0.5),
                     reads=rstd.res, writes=rstd.res)
                for c in range(NKC):
                    T.op(DVE, lambda: nc.vector.scalar_tensor_tensor(
                        out=xT[:, c, :], in0=xT[:, c, :], scalar=small[:, C_GFIN + c:C_GFIN + c + 1],
                        in1=rstd.ap, op0=ALU.mult, op1=ALU.mult),
                        reads=flat_res(x_res[c], rstd, small_res), writes=[x_res[c]])
            for c in range(NKC):
                T.dma(SP, sem_out, lambda: nc.sync.dma_start(out=yT_d[c * 128:(c + 1) * 128, :], in_=xT[:, c, :]),
                      reads=[x_res[c]])
            if not T.plan:
                nc.sync.wait_ge(sem_out.sem, sem_out.cnt)

        def body():
            for (gl, mi, fi) in layers:
                if "mix" in comps:
                    if gl % 2 == 0:
                        fox_layer(gl, mi)
                    else:
                        hgrn_layer(gl, mi)
                if "ffn" in comps:
                    ffn(gl, fi)

        T.plan = True
        body()
        T.plan = False
        setup()
        body()
        finish()
        assert W.i_get == len(W.plan)
    return nc


def _consts():
    c = np.zeros((128, NCONST), np.float32)
    p = np.arange(128)[:, None]
    q = np.arange(128)[None, :]
    c[:, K_IDENT:K_IDENT + 128] = (p == q)
    c[:, K_ONES:K_ONES + 128] = 1.0
    c[:, K_MASKNEG:K_MASKNEG + 128] = np.where(p > q, NEG, 0.0)
    t = np.arange(512)[None, :] % 64
    c[:, K_HMASK:K_HMASK + 512] = (t >= (p % 64))
    tt = np.arange(S)[None, :]
    c[:, K_SCAN:K_SCAN + S] = np.broadcast_to((tt % 64) != 0, (128, S))
    return c


def _pack_small(inp):
    sm = np.zeros((128, NSMALL), np.float32)
    for l in range(DEPTH):
        sm[:, C_GMIX + l * 8:C_GMIX + l * 8 + 8] = inp["norm_mix"][l].reshape(8, 128).T
        sm[:, C_GFFN + l * 8:C_GFFN + l * 8 + 8] = inp["norm_ffn"][l].reshape(8, 128).T
    sm[:, C_GFIN:C_GFIN + 8] = inp["final_norm"].reshape(8, 128).T
    for j in range(2):
        for tap in range(3):
            sm[:, C_CONVW + (j * 3 + tap) * 4:C_CONVW + (j * 3 + tap) * 4 + 4] = inp["conv_w"][j, tap].reshape(4, 128).T
        sm[:, C_CLB + j * 8:C_CLB + j * 8 + 8] = inp["c_lower_bounds"][j].reshape(8, 128).T
        sm[:, C_HNORM + j * 8:C_HNORM + j * 8 + 8] = inp["c_head_norm"][j].reshape(8, 128).T
        sm[0:8, C_FBIAS + j] = inp["fox_f_bias"][j]
    return sm


def _prep_weights(inp):
    ab = inp["ab_w_in"]
    n_ab = ab.shape[0]
    q = ab[:, :, 0:512].reshape(n_ab, D, 8, 64)
    k = ab[:, :, 512:1024].reshape(n_ab, D, 8, 64)
    v = ab[:, :, 1024:1536].reshape(n_ab, D, 8, 64)
    att = np.concatenate([q.reshape(n_ab, D, 4, 128), k.reshape(n_ab, D, 4, 128), v.reshape(n_ab, D, 4, 128)],
                         axis=3)
    f = ab[:, :, 1536:1544]
    ub = ab[:, :, 1544:2056].reshape(n_ab, D, 4, 128)
    uc = ab[:, :, 2056:2568].reshape(n_ab, D, 4, 128)
    ux = ab[:, :, 2568:3080].reshape(n_ab, D, 4, 128)
    conv = np.concatenate([uc, ux, ub], axis=3)
    cw = inp["c_w_in"]
    n_c = cw.shape[0]
    cin = cw.reshape(n_c, D, 4, 8, 128).transpose(0, 1, 3, 2, 4).reshape(n_c, D, 8, 512)
    fw = inp["ffn_w_in"]
    n_f = fw.shape[0]
    fin = fw.reshape(n_f, D, 2, NHC, 128).transpose(0, 1, 3, 2, 4).reshape(n_f, D, NHC, 256)
    c = np.ascontiguousarray
    return {
        "ab_att": c(att), "ab_f": c(f), "ab_conv": c(conv), "ab_out": c(inp["ab_w_out"]),
        "c_in": c(cin), "c_out": c(inp["c_w_out"]),
        "ffn_in": c(fin), "ffn_out": c(inp["ffn_w_out"]),
    }


_NC_CACHE = {}


def run_layers(xT_list, inp, wts, layer_ids, final, comps=("mix", "ffn")):
    n_ab = sum(1 for l in layer_ids if l % 2 == 0)
    n_c = sum(1 for l in layer_ids if l % 2 == 1)
    n_f = len(layer_ids)
    layers = []
    ia = ic = 0
    for i, l in enumerate(layer_ids):
        if l % 2 == 0:
            layers.append((l, ia, i)); ia += 1
        else:
            layers.append((l, ic, i)); ic += 1
    key = (tuple(layer_ids), final, tuple(comps))
    if key not in _NC_CACHE:
        _NC_CACHE[key] = build(layers, final, n_ab, n_c, n_f, comps)
    nc = _NC_CACHE[key]
    ab_ids = [l // 2 for l in layer_ids if l % 2 == 0] or [0]
    c_ids = [l // 2 for l in layer_ids if l % 2 == 1] or [0]
    f_ids = list(layer_ids)
    shared = {
        "small": _pack_small(inp), "consts": _consts(),
        "ab_att": np.ascontiguousarray(wts["ab_att"][ab_ids]), "ab_f": np.ascontiguousarray(wts["ab_f"][ab_ids]),
        "ab_conv": np.ascontiguousarray(wts["ab_conv"][ab_ids]), "ab_out": np.ascontiguousarray(wts["ab_out"][ab_ids]),
        "c_in": np.ascontiguousarray(wts["c_in"][c_ids]), "c_out": np.ascontiguousarray(wts["c_out"][c_ids]),
        "ffn_in": np.ascontiguousarray(wts["ffn_in"][f_ids]), "ffn_out": np.ascontiguousarray(wts["ffn_out"][f_ids]),
    }
    in_maps = [dict(shared, xT=xT_list[b]) for b in range(NCORES)]
    res = run_bass_kernel_spmd(nc, in_maps, core_ids=list(range(NCORES)))
    return [np.asarray(r["yT"]) for r in res.results]


def kernel(**inputs):
    inp = {k: np.asarray(v, dtype=np.float32) for k, v in inputs.items()}
    x = inp["x"]
    wts = _prep_weights(inp)
    xT = [np.ascontiguousarray(x[b].T) for b in range(NCORES)]
    yT = run_layers(xT, inp, wts, [0, 1, 2, 3], True)
    return np.stack([y.T for y in yT], axis=0).astype(np.float32)
```
